# Optimizing a Trainium2 kernel written in Bass

```python
import jax, jax.numpy as jnp
from jax import lax
import numpy as np

D_MODEL = 1024
BATCH = 2
SEQ = 8192
DEPTH = 1

GRID_W = 64
CTX_LEN = 256

HEAD_DIM = 64
N_HEADS = 16
N_KV_HEADS = 4
GROUP = N_HEADS // N_KV_HEADS
ATTN_W = N_HEADS * HEAD_DIM
KV_W = N_KV_HEADS * HEAD_DIM
WINDOW = 128
ATTN_BLOCK = 128
ATTN_SCALE = HEAD_DIM ** -0.5
ROPE_BASE = 10000.0
ROPE_PAIRS = HEAD_DIM // 4

POOL_WINDOWS = (2, 4, 8, 16)
N_POOL_GROUPS = len(POOL_WINDOWS)
POOL_W = D_MODEL
POOL_GROUP_W = POOL_W // N_POOL_GROUPS

IN_W = ATTN_W + 2 * KV_W + POOL_W + 2 * D_MODEL
SPLIT_POINTS = (ATTN_W, ATTN_W + KV_W, ATTN_W + 2 * KV_W,
                ATTN_W + 2 * KV_W + POOL_W, ATTN_W + 2 * KV_W + POOL_W + D_MODEL)

N_EXPERTS = 32
TOP_K = 4
D_FF = D_MODEL
SWIGLU_ALPHA = 1.702
SWIGLU_LIMIT = 7.0
EXPERT_BLOCK = 128

NORM_EPS = 1e-5
NEG_INF = -1e30

kernel_name = 'hybrid_dit_gqa_pool_moe_block'


def rmsnorm(x, g):
    xf = x.astype(jnp.float32)
    y = xf * lax.rsqrt(jnp.mean(xf * xf, axis=-1, keepdims=True) + NORM_EPS)
    return (y * g.astype(jnp.float32)).astype(x.dtype)


def modulate(h, shift, scale):
    return h * (1 + scale) + shift


def apply_axial_rope(t, cos, sin):
    B, L, H, _ = t.shape
    tr = t.reshape(B, L, H, 2, 2, ROPE_PAIRS)
    t1, t2 = tr[..., 0, :], tr[..., 1, :]
    cs = cos[None, :, None].astype(t.dtype)
    sn = sin[None, :, None].astype(t.dtype)
    out = jnp.stack([t1 * cs - t2 * sn, t2 * cs + t1 * sn], axis=-2)
    return out.reshape(t.shape)


def windowed_attention(q, k, v, k_ctx, v_ctx, sink):
    B, L = q.shape[:2]
    Lc = k_ctx.shape[1]
    nblk = L // ATTN_BLOCK
    qb = q.reshape(B, nblk, ATTN_BLOCK, N_KV_HEADS, GROUP, HEAD_DIM).transpose(1, 0, 2, 3, 4, 5)
    pad = ((0, 0), (ATTN_BLOCK, ATTN_BLOCK), (0, 0), (0, 0))

    def band(t):
        tp = jnp.pad(t, pad).reshape(B, nblk + 2, ATTN_BLOCK, N_KV_HEADS, HEAD_DIM)
        tb = jnp.concatenate([tp[:, :-2], tp[:, 1:-1], tp[:, 2:]], axis=2)
        return tb.transpose(1, 0, 2, 3, 4)

    k_band, v_band = band(k), band(v)
    sink_l = sink.reshape(N_KV_HEADS, GROUP).astype(jnp.float32)
    q_off = jnp.arange(ATTN_BLOCK, dtype=jnp.int32)
    k_off = jnp.arange(3 * ATTN_BLOCK, dtype=jnp.int32) - ATTN_BLOCK
    n_loc = 3 * ATTN_BLOCK

    def one_block(args):
        qn, kn, vn, n = args
        s_loc = jnp.einsum('bqhgd,bshd->bhgqs', qn, kn).astype(jnp.float32) * ATTN_SCALE
        s_ctx = jnp.einsum('bqhgd,bchd->bhgqc', qn, k_ctx).astype(jnp.float32) * ATTN_SCALE
        qpos = n * ATTN_BLOCK + q_off
        kpos = n * ATTN_BLOCK + k_off
        valid = ((jnp.abs(qpos[:, None] - kpos[None, :]) <= WINDOW)
                 & (kpos[None, :] >= 0) & (kpos[None, :] < L))
        s_loc = jnp.where(valid, s_loc, NEG_INF)
        sink_b = jnp.broadcast_to(sink_l[None, :, :, None, None], s_loc.shape[:-1] + (1,))
        probs = jax.nn.softmax(jnp.concatenate([s_loc, s_ctx, sink_b], axis=-1), axis=-1)
        p_loc = probs[..., :n_loc].astype(vn.dtype)
        p_ctx = probs[..., n_loc:n_loc + Lc].astype(v_ctx.dtype)
        return (jnp.einsum('bhgqs,bshd->bqhgd', p_loc, vn)
                + jnp.einsum('bhgqc,bchd->bqhgd', p_ctx, v_ctx))

    out = lax.map(one_block, (qb, k_band, v_band, jnp.arange(nblk, dtype=jnp.int32)))
    return out.transpose(1, 0, 2, 3, 4, 5).reshape(B, L, ATTN_W)


def context_attention(q, k, v, sink):
    B, Lc = q.shape[:2]
    qg = q.reshape(B, Lc, N_KV_HEADS, GROUP, HEAD_DIM)
    s = jnp.einsum('bqhgd,bchd->bhgqc', qg, k).astype(jnp.float32) * ATTN_SCALE
    sink_b = jnp.broadcast_to(
        sink.reshape(N_KV_HEADS, GROUP).astype(jnp.float32)[None, :, :, None, None],
        s.shape[:-1] + (1,))
    p = jax.nn.softmax(jnp.concatenate([s, sink_b], axis=-1), axis=-1)[..., :Lc].astype(v.dtype)
    return jnp.einsum('bhgqc,bchd->bqhgd', p, v).reshape(B, Lc, ATTN_W)


def multiscale_pool(u, w_pool, pool_scale):
    B, L, _ = u.shape
    ug = u.reshape(B, L, N_POOL_GROUPS, POOL_GROUP_W).astype(jnp.float32)
    cs = jnp.concatenate([jnp.zeros_like(ug[:, :1]), jnp.cumsum(ug, axis=1)], axis=1)
    half = jnp.array([w // 2 for w in POOL_WINDOWS], dtype=jnp.int32)
    t = jnp.arange(L, dtype=jnp.int32)[:, None]
    lo = jnp.clip(t - half[None, :], 0, L)
    hi = jnp.clip(t + half[None, :], 0, L)
    g = jnp.arange(N_POOL_GROUPS, dtype=jnp.int32)[None, :]
    window_sum = cs[:, hi, g] - cs[:, lo, g]
    mean = window_sum / (hi - lo).astype(jnp.float32)[None, :, :, None]
    diff = (mean - ug).astype(u.dtype)
    mixed = jnp.einsum('blgc,gcd->blgd', diff, w_pool)
    return mixed.reshape(B, L, POOL_W) * pool_scale


def merge_branches(attn_o, pool_o, g_attn, g_pool, w_attn_br, w_pool_br, w_out):
    merged = (jax.nn.sigmoid(g_attn) * (attn_o @ w_attn_br)
              + jax.nn.sigmoid(g_pool) * (pool_o @ w_pool_br))
    return merged @ w_out


def clamped_swiglu(gu):
    gate = jnp.minimum(gu[..., :D_FF], SWIGLU_LIMIT)
    lin = jnp.clip(gu[..., D_FF:], -SWIGLU_LIMIT, SWIGLU_LIMIT)
    return gate * jax.nn.sigmoid(SWIGLU_ALPHA * gate) * (lin + 1)


def moe_ffn(h, w_router, b_router, w_gu, b_gu, w_down, b_down):
    N, D = h.shape
    logits = (h @ w_router + b_router).astype(jnp.float32)
    top_val, top_idx = lax.top_k(logits, TOP_K)
    gate_w = jax.nn.softmax(top_val, axis=-1).astype(h.dtype)
    NK = N * TOP_K
    flat_e = top_idx.reshape(-1)
    order = jnp.argsort(flat_e)
    sorted_e = flat_e[order]
    tok_sorted = (order // TOP_K).astype(jnp.int32)
    counts = jnp.zeros((N_EXPERTS,), jnp.int32).at[flat_e].add(1)
    padded = (counts + EXPERT_BLOCK - 1) // EXPERT_BLOCK * EXPERT_BLOCK
    start = jnp.cumsum(counts) - counts
    pend = jnp.cumsum(padded)
    pstart = pend - padded
    dest = pstart[sorted_e] + (jnp.arange(NK, dtype=jnp.int32) - start[sorted_e])
    num_blocks = -(-NK // EXPERT_BLOCK) + N_EXPERTS
    P = num_blocks * EXPERT_BLOCK
    tok_buf = jnp.full((P,), N, jnp.int32).at[dest].set(tok_sorted)
    h_pad = jnp.concatenate([h, jnp.zeros((1, D), h.dtype)], axis=0)
    x_blocks = h_pad[tok_buf].reshape(num_blocks, EXPERT_BLOCK, D)
    block_start = jnp.arange(num_blocks, dtype=jnp.int32) * EXPERT_BLOCK
    block_e = jnp.minimum(jnp.searchsorted(pend, block_start, side='right'), N_EXPERTS - 1)

    def expert_block(args):
        xb, e = args
        gu = xb @ w_gu[e] + b_gu[e]
        return clamped_swiglu(gu) @ w_down[e] + b_down[e]

    y_buf = lax.map(expert_block, (x_blocks, block_e)).reshape(P, D)
    y_sorted = y_buf[dest] * gate_w.reshape(-1)[order][:, None]
    return jax.ops.segment_sum(y_sorted, tok_sorted, num_segments=N)


def hybrid_layer(x, ctx, rope_cos, rope_sin, c, c_ctx, w_ada, b_ada, norm1_g, norm2_g,
                 w_in, b_in, attn_sink, w_pool, pool_scale, w_attn_br, w_pool_br, w_out,
                 w_router, b_router, w_gu, b_gu, w_down, b_down, update_ctx):
    B, L, D = x.shape
    Lc = ctx.shape[1]
    mod_x = (jax.nn.silu(c) @ w_ada + b_ada)[:, None, :]
    mod_c = jax.nn.silu(c_ctx) @ w_ada + b_ada
    sh1x, sc1x, g1x, sh2x, sc2x, g2x = jnp.split(mod_x, 6, axis=-1)
    sh1c, sc1c, g1c, sh2c, sc2c, g2c = jnp.split(mod_c, 6, axis=-1)

    hx = modulate(rmsnorm(x, norm1_g), sh1x, sc1x)
    hc = modulate(rmsnorm(ctx, norm1_g), sh1c, sc1c)
    qx, kx, vx, ux, gax, gpx = jnp.split(hx @ w_in + b_in, SPLIT_POINTS, axis=-1)
    kv_sl = slice(ATTN_W, ATTN_W + 2 * KV_W)
    kc, vc = jnp.split(hc @ w_in[:, kv_sl] + b_in[kv_sl], 2, axis=-1)
    kc = kc.reshape(B, Lc, N_KV_HEADS, HEAD_DIM)
    vc = vc.reshape(B, Lc, N_KV_HEADS, HEAD_DIM)
    qx = apply_axial_rope(qx.reshape(B, L, N_HEADS, HEAD_DIM), rope_cos, rope_sin)
    kx = apply_axial_rope(kx.reshape(B, L, N_KV_HEADS, HEAD_DIM), rope_cos, rope_sin)
    vx = vx.reshape(B, L, N_KV_HEADS, HEAD_DIM)
    attn_x = windowed_attention(qx, kx, vx, kc, vc, attn_sink)
    pool_x = multiscale_pool(ux, w_pool, pool_scale)
    x = x + g1x * merge_branches(attn_x, pool_x, gax, gpx, w_attn_br, w_pool_br, w_out)
    if update_ctx:
        qc, _, _, uc, gac, gpc = jnp.split(hc @ w_in + b_in, SPLIT_POINTS, axis=-1)
        attn_c = context_attention(qc.reshape(B, Lc, N_HEADS, HEAD_DIM), kc, vc, attn_sink)
        pool_c = multiscale_pool(uc, w_pool, pool_scale)
        ctx = ctx + g1c * merge_branches(attn_c, pool_c, gac, gpc, w_attn_br, w_pool_br, w_out)

    h2x = modulate(rmsnorm(x, norm2_g), sh2x, sc2x).reshape(B * L, D)
    if update_ctx:
        h2c = modulate(rmsnorm(ctx, norm2_g), sh2c, sc2c).reshape(B * Lc, D)
        y = moe_ffn(jnp.concatenate([h2x, h2c], axis=0),
                    w_router, b_router, w_gu, b_gu, w_down, b_down)
        x = x + g2x * y[:B * L].reshape(B, L, D)
        ctx = ctx + g2c * y[B * L:].reshape(B, Lc, D)
    else:
        y = moe_ffn(h2x, w_router, b_router, w_gu, b_gu, w_down, b_down)
        x = x + g2x * y.reshape(B, L, D)
    return x, ctx


def setup_inputs(seed: int = 0) -> dict:
    key = jax.random.key(seed)
    ks = jax.random.split(key, 24)

    def nrm(k, shape, scale):
        return jax.random.normal(k, shape, jnp.float32) * scale

    D = D_MODEL
    return {
        'x': nrm(ks[0], (BATCH, SEQ, D), 1.0),
        'c': nrm(ks[1], (BATCH, D), 1.0),
        'ctx': nrm(ks[2], (BATCH, CTX_LEN, D), 1.0),
        'c_ctx': nrm(ks[3], (D,), 1.0),
        'w_ada': nrm(ks[4], (DEPTH, D, 6 * D), 0.5 * D ** -0.5),
        'b_ada': nrm(ks[5], (DEPTH, 6 * D), 0.02),
        'norm1_g': 1.0 + nrm(ks[6], (DEPTH, D), 0.02),
        'norm2_g': 1.0 + nrm(ks[7], (DEPTH, D), 0.02),
        'w_in': nrm(ks[8], (DEPTH, D, IN_W), D ** -0.5),
        'b_in': nrm(ks[9], (DEPTH, IN_W), 0.02),
        'attn_sink': nrm(ks[10], (DEPTH, N_HEADS), 0.5),
        'w_pool': nrm(ks[11], (DEPTH, N_POOL_GROUPS, POOL_GROUP_W, POOL_GROUP_W), POOL_GROUP_W ** -0.5),
        'pool_scale': 1.0 + nrm(ks[12], (DEPTH, POOL_W), 0.02),
        'w_attn_br': nrm(ks[13], (DEPTH, ATTN_W, D), ATTN_W ** -0.5),
        'w_pool_br': nrm(ks[14], (DEPTH, POOL_W, D), POOL_W ** -0.5),
        'w_out': nrm(ks[15], (DEPTH, D, D), D ** -0.5),
        'w_router': nrm(ks[16], (DEPTH, D, N_EXPERTS), D ** -0.5),
        'b_router': nrm(ks[17], (DEPTH, N_EXPERTS), 0.01),
        'w_gu': nrm(ks[18], (DEPTH, N_EXPERTS, D, 2 * D_FF), D ** -0.5),
        'b_gu': nrm(ks[19], (DEPTH, N_EXPERTS, 2 * D_FF), 0.02),
        'w_down': nrm(ks[20], (DEPTH, N_EXPERTS, D_FF, D), D_FF ** -0.5),
        'b_down': nrm(ks[21], (DEPTH, N_EXPERTS, D), 0.02),
        'final_g': 1.0 + nrm(ks[22], (D,), 0.02),
    }


def reference(x, c, ctx, c_ctx, w_ada, b_ada, norm1_g, norm2_g, w_in, b_in, attn_sink,
              w_pool, pool_scale, w_attn_br, w_pool_br, w_out, w_router, b_router,
              w_gu, b_gu, w_down, b_down, final_g):
    L = x.shape[1]
    ROWS = L // GRID_W
    row = jnp.broadcast_to(jnp.arange(ROWS, dtype=jnp.float32)[:, None], (ROWS, GRID_W)).reshape(-1)
    col = jnp.broadcast_to(jnp.arange(GRID_W, dtype=jnp.float32)[None, :], (ROWS, GRID_W)).reshape(-1)
    inv_freq = ROPE_BASE ** (-jnp.arange(ROPE_PAIRS, dtype=jnp.float32) / ROPE_PAIRS)
    ang = jnp.stack([row[:, None] * inv_freq, col[:, None] * inv_freq], axis=1)
    rope_cos, rope_sin = jnp.cos(ang), jnp.sin(ang)
    for i in range(DEPTH):
        x, ctx = hybrid_layer(
            x, ctx, rope_cos, rope_sin, c, c_ctx, w_ada[i], b_ada[i], norm1_g[i], norm2_g[i],
            w_in[i], b_in[i], attn_sink[i], w_pool[i], pool_scale[i], w_attn_br[i],
            w_pool_br[i], w_out[i], w_router[i], b_router[i], w_gu[i], b_gu[i],
            w_down[i], b_down[i], update_ctx=(i + 1 < DEPTH))
    return rmsnorm(x, final_g)
```

```python
import contextlib
import numpy as np
import concourse.bass as bass
import concourse.mybir as mybir
from concourse.bass_utils import run_bass_kernel_spmd

F32 = mybir.dt.float32
BF16 = mybir.dt.bfloat16
AF = mybir.ActivationFunctionType
ALU = mybir.AluOpType
AX = mybir.AxisListType

D = 1024
NE = 32
HALF = 1024
NT = 8
NTH = 10
DEBUG = False
import os
NEXP = int(os.environ.get('NEXP', '32'))


class Buf:
    __slots__ = ("name", "lastw", "readers")

    def __init__(self, name):
        self.name = name
        self.lastw = None
        self.readers = []


class Op:
    __slots__ = ("eng", "fn", "deps", "kind", "dsem", "ndep", "tok")

    def __init__(self, eng, fn, kind, dsem=None):
        self.eng = eng
        self.fn = fn
        self.kind = kind
        self.dsem = dsem
        self.deps = []
        self.ndep = 0
        self.tok = None


class Prog:
    ENGS = ("pe", "act", "dve", "pool", "sp")

    def __init__(self):
        self.ops = {e: [] for e in self.ENGS}
        self.dma_sems = []

    def _add(self, op, reads, writes):
        deps = []
        for b in reads:
            w = b.lastw
            if w is not None:
                deps.append(w)
        for b in writes:
            w = b.lastw
            if w is not None:
                if w.kind == 'c' and op.kind == 'c' and w.eng == op.eng:
                    pass
                else:
                    deps.append(w)
            for r in b.readers:
                if r.kind == 'c' and op.kind == 'c' and r.eng == op.eng:
                    continue
                deps.append(r)
        seen = set()
        for d in deps:
            if d is op or id(d) in seen:
                continue
            seen.add(id(d))
            if d.kind == 'c' and op.kind == 'c' and d.eng == 'pe' and op.eng == 'pe':
                continue
            op.deps.append(d)
            d.ndep += 1
        for b in writes:
            b.lastw = op
            b.readers = []
        for b in reads:
            if b.lastw is not op:
                b.readers.append(op)
        self.ops[op.eng].append(op)
        return op

    def c(self, eng, fn, reads=(), writes=()):
        return self._add(Op(eng, fn, 'c'), reads, writes)

    def dma(self, eng, fn, dsem, reads=(), writes=()):
        if dsem not in self.dma_sems:
            self.dma_sems.append(dsem)
        return self._add(Op(eng, fn, 'd', dsem), reads, writes)

    def emit(self, nc, final_ops=()):
        CH = 8000
        NPOOL = 16
        with contextlib.ExitStack() as st:
            esems = {}
            sems = {}
            for e in self.ENGS:
                n = 0
                nd = 0
                for op in self.ops[e]:
                    if op.kind == 'd':
                        key = ("dma", e, nd % NPOOL)
                        nd += 1
                        if key not in sems:
                            sems[key] = [st.enter_context(nc.semaphore("d_%s%d" % (e, key[2]))), 0]
                        op.dsem = (key, sems[key][1])
                        sems[key][1] += 16
                        op.tok = (key, sems[key][1])
                    elif op.ndep > 0:
                        n += 1
                        key = (e, (n - 1) // CH)
                        if key not in sems:
                            sems[key] = [st.enter_context(nc.semaphore("e_%s%d" % key)), 0]
                        op.tok = (key, (n - 1) % CH + 1)
            block = st.enter_context(nc.Block())

            def run(eng_name, eng):
                waited = {}
                for op in self.ops[eng_name]:
                    need = {}
                    for d in op.deps:
                        k, v = d.tok
                        if waited.get(k, 0) < v and need.get(k, 0) < v:
                            need[k] = v
                    if op.kind == 'd':
                        k, v = op.dsem
                        if v > 0 and waited.get(k, 0) < v and need.get(k, 0) < v:
                            need[k] = v
                    for k, v in need.items():
                        waited[k] = v
                        eng.wait_ge(sems[k][0], v)
                    ins = op.fn(eng)
                    if op.kind == 'd':
                        ins.then_inc(sems[op.tok[0]][0], 16)
                    elif op.tok is not None:
                        ins.then_inc(sems[op.tok[0]][0], 1)
                if eng_name == 'sp':
                    for d in final_ops:
                        k, v = d.tok
                        eng.wait_ge(sems[k][0], v)

            block.tensor(lambda eng: run('pe', eng))
            block.scalar(lambda eng: run('act', eng))
            block.vector(lambda eng: run('dve', eng))
            block.gpsimd(lambda eng: run('pool', eng))
            block.sync(lambda eng: run('sp', eng))
            self.nsems = len(sems)


class T:
    __slots__ = ("name", "ap", "b", "start", "end", "flat", "uid")


class Arena:
    def __init__(self, nc, nbytes):
        self.nbytes = nbytes
        self.t = nc.sbuf_tensor("arena", [128, nbytes // 2], BF16).__enter__()
        self.free = [(0, nbytes)]
        self.grave = []
        self.live = {}
        self.peak = 0
        self.uid = 0

    def alloc(self, name, shape, dt, nb=1, parts=128, top=False):
        n = 1
        for s in shape:
            n *= s
        nbytes = n * (4 if dt == F32 else 2)
        nbytes = (nbytes + 63) // 64 * 64
        order = range(len(self.free) - 1, -1, -1) if top else range(len(self.free))
        for i in order:
            s, e = self.free[i]
            if e - s >= nbytes:
                if top:
                    self.free[i] = (s, e - nbytes)
                    s = e - nbytes
                else:
                    self.free[i] = (s + nbytes, e)
                if self.free[i][0] == self.free[i][1]:
                    del self.free[i]
                break
        else:
            raise RuntimeError("arena OOM for %s (%d B); live=%s" % (name, nbytes, sorted(
                (v_.end - v_.start, k) for k, v_ in self.live.items())))
        t = T()
        self.uid += 1
        t.uid = self.uid
        t.name = "%s_%d" % (name, self.uid)
        t.start, t.end = s, s + nbytes
        self.peak = max(self.peak, t.end)
        v = self.t[0:parts, s // 2:(s + nbytes) // 2]
        if dt != BF16:
            v = v.bitcast(dt)
        v = v[:, 0:n]
        t.flat = v
        if len(shape) == 2:
            v = v.rearrange("p (a b) -> p a b", a=shape[0])
        elif len(shape) == 3:
            v = v.rearrange("p (a b c) -> p a b c", a=shape[0], b=shape[1])
        elif len(shape) == 4:
            v = v.rearrange("p (a b c d) -> p a b c d", a=shape[0], b=shape[1], c=shape[2])
        t.ap = v
        t.b = [Buf("%s.%d" % (name, i)) for i in range(nb)]
        inh = []
        for (gs, ge, bufs) in self.grave:
            if gs < t.end and ge > t.start:
                for gb in bufs:
                    if gb.lastw is not None:
                        inh.append(gb.lastw)
                    inh.extend(gb.readers)
        if inh:
            seen = set()
            u = []
            for o in inh:
                if id(o) not in seen:
                    seen.add(id(o))
                    u.append(o)
            for b in t.b:
                b.readers = list(u)
        self.live[t.name] = t
        return t

    def release(self, *ts):
        for t in ts:
            del self.live[t.name]
            self.grave.append((t.start, t.end, t.b))
            self.free.append((t.start, t.end))
        self.free.sort()
        m = []
        for s, e in self.free:
            if m and m[-1][1] == s:
                m[-1] = (m[-1][0], e)
            else:
                m.append((s, e))
        self.free = m


class _Stop(Exception):
    pass


def build_program(stop=None, dumps=None):
    nc = bass.Bass("TRN2", target_bir_lowering=False)
    dumps = {} if dumps is None else dumps

    def phase_end(name, hf, env):
        if stop is not None and stop == (name, hf):
            for dn, (fn_ap, shape) in env.items():
                dt_ = nc.dram_tensor("dbg_" + dn, list(shape), F32, kind="ExternalOutput").ap()
                t_, bufs_ = fn_ap
                final_ops.append(P.dma('pool', lambda e, dt_=dt_, t_=t_: e.dma_start(out=dt_, in_=t_), "dbg", reads=bufs_))
                dumps[dn] = shape
            raise _Stop()

    def din(name, shape):
        return nc.dram_tensor(name, list(shape), F32, kind="ExternalInput").ap()

    xh = din("xh", [2304, D])
    ctxd = din("ctx", [256, D])
    ccols = din("ccols", [128, 16])
    w_ada = din("w_ada", [D, 6 * D])
    b_ada = din("b_ada", [1, 6 * D])
    n1g = din("n1g", [1, D])
    n2g = din("n2g", [1, D])
    fg = din("fg", [1, D])
    w_in = din("w_in", [D, 4608])
    b_in = din("b_in", [1, 4608])
    b_in_cols = din("b_in_cols", [128, 24])
    sink = din("sink", [1, 16])
    w_pool = din("w_pool", [4, 256, 256])
    pscale_cols = din("pscale_cols", [128, 8])
    w_abr = din("w_abr", [D, D])
    w_pbr = din("w_pbr", [D, D])
    w_outd = din("w_out", [D, D])
    w_router = din("w_router", [D, NE])
    b_router = din("b_router", [1, NE])
    w_gu = din("w_gu", [NE, D, 2 * D])
    b_gu_cols = din("b_gu_cols", [128, NE * 16])
    w_down = din("w_down", [NE, D, D])
    b_down = din("b_down", [NE, D])
    identd = din("ident", [128, 128])
    masksd = din("masks", [128, 4 * 512])
    ropec = din("ropec", [128, 18 * 64])
    ropes = din("ropes", [128, 18 * 64])
    pcorr = din("pcorr", [1, 2 * 4 * 16])
    uval = din("uval", [1, 4])
    rowmaskd = din("rowmask", [128, 2])
    m4d = din("m4", [128, 512])
    outd = nc.dram_tensor("out", [2048, D], F32, kind="ExternalOutput").ap()
    modscr = nc.dram_tensor("modscr", [2, 6 * D], F32, kind="Internal").ap()
    dbg_out = {}

    P = Prog()
    ar = Arena(nc, 196608 - 16512 - 64)
    A = ar.alloc
    banks = [nc.psum_tensor("bank%d" % i, [128, 512], F32).__enter__() for i in range(8)]
    PB = [Buf("bank%d" % i) for i in range(8)]

    def bk(i):
        return banks[i][:]

    def bkb(i):
        return banks[i][:].bitcast(BF16)

    def C(eng, fn, r=(), w=()):
        return P.c(eng, fn, reads=r, writes=w)

    def mmf(out, lhsT, rhs, start=True, stop=True):
        return lambda e: e.matmul(out, lhsT=lhsT, rhs=rhs, start=start, stop=stop)

    def DMA(eng, out, in_, dsem, r=(), w=()):
        return P.dma(eng, lambda e: e.dma_start(out=out, in_=in_), dsem, reads=r, writes=w)

    def load(t, in_, cast=False, flat=False):
        return DMA('pool' if cast else 'sp', t.flat if flat else t.ap, in_, "L_" + t.name, w=t.b)

    final_ops = []
    modB = [[Buf("mod%d_%d" % (w, j)) for j in range(6)] for w in range(2)]
    x1B = [Buf("x1d%d" % i) for i in range(16)]

    try:
        ident = A("ident", [128], BF16)
        load(ident, identd[:, :], cast=True)
        onesb = A("onesb", [128], BF16)
        C('pool', lambda e: e.memset(onesb.ap, 1.0), w=onesb.b)
        onesf = A("onesf", [128], F32)
        C('pool', lambda e: e.memset(onesf.ap, 1.0), w=onesf.b)
        epst = A("eps", [1], F32)
        C('pool', lambda e: e.memset(epst.ap, 1e-5), w=epst.b)
        bcols = A("bcols", [24], F32)
        load(bcols, b_in_cols[:, :])
        pscol = A("pscol", [8], F32)
        load(pscol, pscale_cols[:, :])
        pcor = A("pcor", [2, 4, 16], F32)
        load(pcor, pcorr.partition_broadcast(128), flat=True)
        uv = A("uval", [4], F32)
        load(uv, uval.partition_broadcast(128))
        brt = A("brt", [NE], F32)
        load(brt, b_router.partition_broadcast(128))
        wr = A("wr", [8, NE], BF16)
        load(wr, w_router.rearrange("(k p) n -> p k n", p=128), cast=True)
        bguc = A("bguc", [NE, 16], F32)
        load(bguc, b_gu_cols.rearrange("p (a b) -> p a b", a=NE))
        C('dve', lambda e: e.tensor_scalar(out=bguc.ap[:, :, 8:16], in0=bguc.ap[:, :, 8:16], scalar1=1.0, scalar2=None,
                                           op0=ALU.add), r=bguc.b, w=bguc.b)
        bdn = A("bdn", [D], BF16)
        C('pool', lambda e: e.memset(bdn.ap, 0.0), w=bdn.b)
        DMA('pool', bdn.ap[0:NE, :], b_down[:, :], "L_bdn", w=bdn.b)
        cc = A("cc", [8, 2], F32)
        load(cc, ccols.rearrange("p (k w) -> p k w", w=2))
        scb = A("scb", [8, 2], BF16)
        C('act', lambda e: e.activation(out=scb.ap, in_=cc.ap, func=AF.Silu), r=cc.b, w=scb.b)
        badb = A("badb", [6 * D], F32, parts=2)
        load(badb, b_ada.partition_broadcast(2))
        ng = A("ng", [2, D], F32, parts=2)
        DMA('sp', ng.ap[:, 0, :], n1g.partition_broadcast(2), "L_ng0", w=ng.b)
        DMA('sp', ng.ap[:, 1, :], n2g.partition_broadcast(2), "L_ng1", w=ng.b)
        wad = A("wad", [2, 8, D], BF16, nb=2)
        mrow = A("mrow", [2, D], F32, nb=2, parts=2)
        for j in range(6):
            s = j % 2
            DMA('pool', wad.ap[:, s], w_ada[:, j * D:(j + 1) * D].rearrange("(k p) n -> p k n", p=128),
                "L_%s_%d" % (wad.name, s), w=[wad.b[s]])
            for hh in range(2):
                for k in range(8):
                    C('pe', mmf(banks[hh][0:2, :], scb.ap[:, k, :], wad.ap[:, s, k, hh * 512:(hh + 1) * 512],
                                k == 0, k == 7), r=[wad.b[s]] + scb.b, w=[PB[hh]])
                C('dve', lambda e, hh=hh, s=s, j=j: e.tensor_tensor(
                    out=mrow.ap[:, s, hh * 512:(hh + 1) * 512], in0=banks[hh][0:2, :],
                    in1=badb.ap[:, j * D + hh * 512: j * D + (hh + 1) * 512], op=ALU.add),
                  r=[PB[hh]] + badb.b, w=[mrow.b[s]])
            if j in (1, 4):
                gi = 0 if j == 1 else 1
                C('dve', lambda e, s=s, gi=gi: e.scalar_tensor_tensor(
                    out=mrow.ap[:, s, :], in0=mrow.ap[:, s, :], scalar=1.0, in1=ng.ap[:, gi, :],
                    op0=ALU.add, op1=ALU.mult), r=[mrow.b[s]] + ng.b, w=[mrow.b[s]])
            DMA('sp', modscr[:, j * D:(j + 1) * D], mrow.ap[:, s, :], "S_%s_%d" % (mrow.name, s), r=[mrow.b[s]],
                w=[modB[0][j], modB[1][j]])
        ar.release(cc, scb, badb, ng, wad, mrow)

        def load_mod(name, wsel, j):
            t = A(name, [D], F32)
            DMA('sp', t.ap, modscr[wsel:wsel + 1, j * D:(j + 1) * D].partition_broadcast(128), "L_" + t.name,
                r=[modB[wsel][j]], w=t.b)
            return t

        def norm_mod_T(src_ap, src_bufs, Ab, shb, stat, si, tmpf, tmpb, trb, dstT_ap, dst_bufs):
            ssq = stat.ap[:, si, 0:1]
            rstd = stat.ap[:, si, 1:2]
            sb = [stat.b[si]]
            C('act', lambda e: e.activation(out=tmpf[0], in_=src_ap, func=AF.Square, accum_out=ssq),
              r=src_bufs, w=[tmpf[1]] + sb)
            C('act', lambda e: e.activation(out=rstd, in_=ssq, func=AF.Sqrt, scale=1.0 / D, bias=epst.ap[:, 0:1]),
              r=sb + epst.b, w=sb)
            C('dve', lambda e: e.reciprocal(out=rstd, in_=rstd), r=sb, w=sb)
            C('dve', lambda e: e.scalar_tensor_tensor(out=tmpf[0], in0=src_ap, scalar=rstd, in1=Ab.ap,
                                                      op0=ALU.mult, op1=ALU.mult),
              r=list(src_bufs) + sb + Ab.b, w=[tmpf[1]])
            if shb is not None:
                C('dve', lambda e: e.tensor_tensor(out=tmpb[0], in0=tmpf[0], in1=shb.ap, op=ALU.add),
                  r=[tmpf[1]] + shb.b, w=[tmpb[1]])
            else:
                return
            for k in range(8):
                C('pe', lambda e, k=k: e.transpose(out=bkb(trb)[:, k * 128:(k + 1) * 128],
                                                   in_=tmpb[0][:, k * 128:(k + 1) * 128], identity=ident.ap),
                  r=[tmpb[1]] + ident.b, w=[PB[trb]])
            C('act', lambda e: e.activation(out=dstT_ap, in_=bkb(trb).rearrange("p (k t) -> p k t", k=8), func=AF.Copy),
              r=[PB[trb]], w=dst_bufs)

        def tiles_of(a, b):
            return list(range(a // 128, (b - 1) // 128 + 1))

        for hf in range(2):
            hbase = hf * HALF
            A1b = load_mod("A1b", 0, 1)
            sh1b = load_mod("sh1b", 0, 0)
            A1cb = load_mod("A1cb", 1, 1)
            sh1cb = load_mod("sh1cb", 1, 0)
            hT = A("hT", [8, NTH * 128], BF16, nb=NTH, top=True)
            hcT = A("hcT", [8, 256], BF16, nb=2)
            xs1 = A("xs1", [2, D], F32, nb=2)
            tf1 = A("tf1", [2, D], F32, nb=2)
            tb1 = A("tb1", [2, D], BF16, nb=2)
            st1 = A("st1", [NTH + 2, 2], F32, nb=NTH + 2)
            for i in range(NTH + 2):
                s = i % 2
                if i < NTH:
                    src = xh[hbase + i * 128: hbase + (i + 1) * 128, :]
                    dst, dbuf, Ab, shb = hT.ap[:, :, i * 128:(i + 1) * 128], [hT.b[i]], A1b, sh1b
                else:
                    ci = i - NTH
                    src = ctxd[ci * 128:(ci + 1) * 128, :]
                    dst, dbuf, Ab, shb = hcT.ap[:, :, ci * 128:(ci + 1) * 128], [hcT.b[ci]], A1cb, sh1cb
                DMA('sp', xs1.ap[:, s], src, "L_%s_%d" % (xs1.name, s), w=[xs1.b[s]])
                norm_mod_T(xs1.ap[:, s], [xs1.b[s]], Ab, shb, st1, i, (tf1.ap[:, s], tf1.b[s]), (tb1.ap[:, s], tb1.b[s]),
                           6 + s, dst, dbuf)
            ar.release(A1b, sh1b, A1cb, sh1cb, xs1, tf1, tb1, st1)
            phase_end('n1', hf, {'hT': ((hT.ap, hT.b), [128, 8, NTH * 128]), 'hcT': ((hcT.ap, hcT.b), [128, 8, 256])})

            poolT = A("poolT", [8, HALF], BF16, nb=8, top=True)
            NU = HALF + 16
            uT = A("uT", [2, NU], F32)
            T1 = A("T1", [2, NU], F32)
            T2 = A("T2", [2, NU], F32)
            dT = A("dT", [2, HALF], BF16)
            wu = A("wu", [2, 8, 256], BF16, nb=2)
            wpool = A("wpool", [4, 2, 256], BF16)
            load(wpool, w_pool.rearrange("g (kc p) n -> p g kc n", p=128), cast=True)
            for g in range(4):
                s = g % 2
                DMA('pool', wu.ap[:, s], w_in[:, 1536 + g * 256: 1536 + (g + 1) * 256].rearrange("(k p) n -> p k n", p=128),
                    "L_%s_%d" % (wu.name, s), w=[wu.b[s]])
                bi = 0
                for mc in range(2):
                    for (a, b) in ((0, 512), (512, 1024), (1024, NU)):
                        pb = bi % 4
                        bi += 1
                        n = b - a
                        for k in range(8):
                            C('pe', mmf(bk(pb)[:, 0:n], wu.ap[:, s, k, mc * 128:(mc + 1) * 128],
                                        hT.ap[:, k, 120 + a:120 + b], k == 0, k == 7),
                              r=[wu.b[s]] + [hT.b[i] for i in tiles_of(120 + a, 120 + b)], w=[PB[pb]])
                        C('act', lambda e, pb=pb, n=n, mc=mc, a=a, b=b, g=g: e.activation(
                            out=uT.ap[:, mc, a:b], in_=bk(pb)[:, 0:n], func=AF.Identity,
                            bias=bcols.ap[:, g * 2 + mc: g * 2 + mc + 1]), r=[PB[pb]] + bcols.b, w=uT.b)
                C('dve', lambda e: e.tensor_scalar(out=uT.ap[:, :, 0:8], in0=uT.ap[:, :, 0:8],
                                                   scalar1=uv.ap[:, 2 * hf:2 * hf + 1], scalar2=None, op0=ALU.mult),
                  r=uT.b + uv.b, w=uT.b)
                C('dve', lambda e: e.tensor_scalar(out=uT.ap[:, :, NU - 8:NU], in0=uT.ap[:, :, NU - 8:NU],
                                                   scalar1=uv.ap[:, 2 * hf + 1:2 * hf + 2], scalar2=None, op0=ALU.mult),
                  r=uT.b + uv.b, w=uT.b)
                cur = uT
                tmps = [T1, T2]
                for n_ in range(1, g + 2):
                    d_ = 2 ** (n_ - 1)
                    lo = 2 ** n_ - 1
                    nxt = tmps[(n_ - 1) % 2]
                    C('dve', lambda e, cur=cur, nxt=nxt, d_=d_, lo=lo: e.tensor_tensor(
                        out=nxt.ap[:, :, lo:NU], in0=cur.ap[:, :, lo:NU], in1=cur.ap[:, :, lo - d_:NU - d_], op=ALU.add),
                      r=cur.b, w=nxt.b)
                    cur = nxt
                hw = 2 ** g
                wwin = 2 * hw
                off = 8 + hw - 1
                C('dve', lambda e, cur=cur, off=off, g=g: e.tensor_tensor(
                    out=cur.ap[:, :, off:off + 8], in0=cur.ap[:, :, off:off + 8],
                    in1=pcor.ap[:, hf, g, 0:8].unsqueeze(1).broadcast_to([128, 2, 8]), op=ALU.mult),
                  r=cur.b + pcor.b, w=cur.b)
                C('dve', lambda e, cur=cur, off=off, g=g: e.tensor_tensor(
                    out=cur.ap[:, :, off + HALF - 8:off + HALF], in0=cur.ap[:, :, off + HALF - 8:off + HALF],
                    in1=pcor.ap[:, hf, g, 8:16].unsqueeze(1).broadcast_to([128, 2, 8]), op=ALU.mult),
                  r=cur.b + pcor.b, w=cur.b)
                C('dve', lambda e, cur=cur, off=off, wwin=wwin: e.scalar_tensor_tensor(
                    out=dT.ap, in0=cur.ap[:, :, off:off + HALF], scalar=1.0 / wwin, in1=uT.ap[:, :, 8:8 + HALF],
                    op0=ALU.mult, op1=ALU.subtract), r=cur.b + uT.b, w=dT.b)
                for mc in range(2):
                    for tc in range(2):
                        pb = 4 + (mc * 2 + tc) % 2
                        for kc in range(2):
                            C('pe', mmf(bk(pb), wpool.ap[:, g, kc, mc * 128:(mc + 1) * 128],
                                        dT.ap[:, kc, tc * 512:(tc + 1) * 512], kc == 0, kc == 1),
                              r=wpool.b + dT.b, w=[PB[pb]])
                        C('act', lambda e, pb=pb, g=g, mc=mc, tc=tc: e.activation(
                            out=poolT.ap[:, 2 * g + mc, tc * 512:(tc + 1) * 512], in_=bk(pb), func=AF.Identity,
                            scale=pscol.ap[:, 2 * g + mc:2 * g + mc + 1]), r=[PB[pb]] + pscol.b, w=[poolT.b[2 * g + mc]])
            ar.release(uT, T1, T2, dT, wu, wpool)
            phase_end('pool', hf, {'poolT': ((poolT.ap, poolT.b), [128, 8, HALF])})

            masks = A("masks", [4, 512], BF16)
            load(masks, masksd.rearrange("p (a b) -> p a b", a=4), cast=True)
            rc = A("ropec", [18, 64], F32)
            load(rc, ropec.rearrange("p (a b) -> p a b", a=18))
            rs_ = A("ropes", [18, 64], F32)
            load(rs_, ropes.rearrange("p (a b) -> p a b", a=18))
            bqkv = A("bqkv", [1536], F32)
            load(bqkv, b_in[0:1, 0:1536].partition_broadcast(128))
            rowm = A("rowm", [2], F32)
            load(rowm, rowmaskd[:, :])
            m4 = A("m4", [512], F32)
            load(m4, m4d[:, :])
            esk = A("esk", [16], F32)
            load(esk, sink.partition_broadcast(128))
            C('act', lambda e, esk=esk: e.activation(out=esk.ap, in_=esk.ap, func=AF.Exp), r=esk.b, w=esk.b)
            eskB = A("eskB", [16, 128], F32)
            for h in range(16):
                C('dve', lambda e, h=h, esk=esk, eskB=eskB: e.tensor_scalar(
                    out=eskB.ap[:, h, :], in0=onesf.ap, scalar1=esk.ap[:, h:h + 1], scalar2=None, op0=ALU.mult),
                  r=esk.b + onesf.b, w=eskB.b)
            kT = A("kT", [4, NTH * 128], BF16, nb=NTH)
            vd = A("vd", [NTH, 4, 2, 64], BF16, nb=NTH)
            kcT = A("kcT", [4, 256], BF16, nb=2)
            vcd = A("vcd", [2, 4, 2, 64], BF16, nb=2)
            wkv = A("wkv", [8, 512], BF16)
            load(wkv, w_in[:, 1024:1536].rearrange("(k p) n -> p k n", p=128), cast=True)
            kvf = A("kvf", [2, 512], F32, nb=2)
            ra = A("ra", [2, 256], F32, nb=2)
            rb = A("rb", [2, 256], F32, nb=2)
            krd = A("krd", [2, 4, 2, 64], BF16, nb=2)
            for i in range(NTH + 2):
                s = i % 2
                isx = i < NTH
                ci = i - NTH
                gt = hf * 8 + i
                for k in range(8):
                    lh = hT.ap[:, k, i * 128:(i + 1) * 128] if isx else hcT.ap[:, k, ci * 128:(ci + 1) * 128]
                    C('pe', mmf(bk(s), lh, wkv.ap[:, k, :], k == 0, k == 7),
                      r=[hT.b[i] if isx else hcT.b[ci]] + wkv.b, w=[PB[s]])
                C('dve', lambda e, s=s: e.tensor_tensor(out=kvf.ap[:, s], in0=bk(s), in1=bqkv.ap[:, 1024:1536], op=ALU.add),
                  r=[PB[s]] + bqkv.b, w=[kvf.b[s]])
                vdst = vd.ap[:, i] if isx else vcd.ap[:, ci]
                vbuf = [vd.b[i]] if isx else [vcd.b[ci]]
                vsrc = kvf.ap[:, s, 256:512].rearrange("p (g d) -> p g d", g=4)
                C('act', lambda e, vdst=vdst, vsrc=vsrc: e.activation(out=vdst[:, :, 0, :], in_=vsrc, func=AF.Copy),
                  r=[kvf.b[s]], w=vbuf)
                C('act', lambda e, vdst=vdst, vsrc=vsrc: e.activation(out=vdst[:, :, 1, :], in_=vsrc, func=AF.Copy),
                  r=[kvf.b[s]], w=vbuf)
                ksrc = kvf.ap[:, s, 0:256]
                if isx:
                    k3 = ksrc.rearrange("p (g d) -> p g d", g=4)
                    k5 = ksrc.rearrange("p (g a h q) -> p g a h q", g=4, a=2, h=2)
                    ra3 = ra.ap[:, s].rearrange("p (g d) -> p g d", g=4)
                    rb5 = rb.ap[:, s].rearrange("p (g a h q) -> p g a h q", g=4, a=2, h=2)
                    sn4 = rs_.ap[:, gt].rearrange("p (a h q) -> p a h q", a=2, h=2)
                    C('dve', lambda e, k3=k3, ra3=ra3, gt=gt: e.tensor_tensor(
                        out=ra3, in0=k3, in1=rc.ap[:, gt].unsqueeze(1).broadcast_to([128, 4, 64]), op=ALU.mult),
                      r=[kvf.b[s]] + rc.b, w=[ra.b[s]])
                    for hh in range(2):
                        C('dve', lambda e, k5=k5, rb5=rb5, sn4=sn4, hh=hh: e.tensor_tensor(
                            out=rb5[:, :, :, hh, :], in0=k5[:, :, :, 1 - hh, :],
                            in1=sn4[:, :, hh, :].unsqueeze(1).broadcast_to([128, 4, 2, 16]), op=ALU.mult),
                          r=[kvf.b[s]] + rs_.b, w=[rb.b[s]])
                    for dd in range(2):
                        C('dve', lambda e, ra3=ra3, s=s, dd=dd: e.tensor_tensor(
                            out=krd.ap[:, s, :, dd, :], in0=ra3, in1=rb.ap[:, s].rearrange("p (g d) -> p g d", g=4),
                            op=ALU.add), r=[ra.b[s], rb.b[s]], w=[krd.b[s]])
                else:
                    k3 = ksrc.rearrange("p (g d) -> p g d", g=4)
                    for dd in range(2):
                        C('dve', lambda e, k3=k3, s=s, dd=dd: e.tensor_copy(out=krd.ap[:, s, :, dd, :], in_=k3),
                          r=[kvf.b[s]], w=[krd.b[s]])
                trb = 6 + s
                for g in range(4):
                    C('pe', lambda e, g=g, s=s, trb=trb: e.transpose(
                        out=bkb(trb)[:, g * 128:(g + 1) * 128],
                        in_=krd.ap[:, s, g].rearrange("p a d -> p (a d)"), identity=ident.ap),
                      r=[krd.b[s]] + ident.b, w=[PB[trb]])
                kdst = kT.ap[:, :, i * 128:(i + 1) * 128] if isx else kcT.ap[:, :, ci * 128:(ci + 1) * 128]
                C('act', lambda e, kdst=kdst, trb=trb: e.activation(
                    out=kdst, in_=bkb(trb)[:, 0:512].rearrange("p (g t) -> p g t", g=4), func=AF.Copy),
                  r=[PB[trb]], w=[kT.b[i]] if isx else [kcT.b[ci]])
            ar.release(wkv, kvf, ra, rb, krd, hcT)
            phase_end('kv', hf, {'kT': ((kT.ap, kT.b), [128, 4, NTH * 128]), 'vd': ((vd.ap, vd.b), [128, NTH, 4, 2, 64]), 'kcT': ((kcT.ap, kcT.b), [128, 4, 256])})

            attnT = A("attnT", [8, HALF], BF16, nb=NT, top=True)
            wq = A("wq", [8, D], BF16)
            load(wq, w_in[:, 0:1024].rearrange("(k p) n -> p k n", p=128), cast=True)
            qf = A("qf", [D], F32)
            qa = A("qa", [D], F32)
            qb = A("qb", [D], F32)
            qr = A("qr", [D], BF16)
            qT = A("qT", [2, 2, 8, 128], BF16, nb=2)
            prod = A("prod", [512], F32)
            PT = A("PT", [3, 5, 512], BF16, nb=3)
            rden = A("rden", [512], F32)
            sbi = [0]

            def att_prologue(t):
                i = t + 1
                gt = hf * 8 + i
                qs = t % 2
                for hh in range(2):
                    for k in range(8):
                        C('pe', mmf(bk(hh), hT.ap[:, k, i * 128:(i + 1) * 128], wq.ap[:, k, hh * 512:(hh + 1) * 512],
                                    k == 0, k == 7), r=[hT.b[i]] + wq.b, w=[PB[hh]])
                    C('dve', lambda e, hh=hh: e.tensor_tensor(out=qf.ap[:, hh * 512:(hh + 1) * 512], in0=bk(hh),
                                                              in1=bqkv.ap[:, hh * 512:(hh + 1) * 512], op=ALU.add),
                      r=[PB[hh]] + bqkv.b, w=qf.b)
                q3 = qf.ap.rearrange("p (g d) -> p g d", g=16)
                q5 = qf.ap.rearrange("p (g a h q) -> p g a h q", g=16, a=2, h=2)
                qa3 = qa.ap.rearrange("p (g d) -> p g d", g=16)
                qb5 = qb.ap.rearrange("p (g a h q) -> p g a h q", g=16, a=2, h=2)
                sn4 = rs_.ap[:, gt].rearrange("p (a h q) -> p a h q", a=2, h=2)
                C('dve', lambda e, q3=q3, qa3=qa3, gt=gt: e.tensor_tensor(
                    out=qa3, in0=q3, in1=rc.ap[:, gt].unsqueeze(1).broadcast_to([128, 16, 64]), op=ALU.mult),
                  r=qf.b + rc.b, w=qa.b)
                for hh in range(2):
                    C('dve', lambda e, q5=q5, qb5=qb5, sn4=sn4, hh=hh: e.tensor_tensor(
                        out=qb5[:, :, :, hh, :], in0=q5[:, :, :, 1 - hh, :],
                        in1=sn4[:, :, hh, :].unsqueeze(1).broadcast_to([128, 16, 2, 16]), op=ALU.mult),
                      r=qf.b + rs_.b, w=qb.b)
                C('dve', lambda e: e.tensor_tensor(out=qr.ap, in0=qa.ap, in1=qb.ap, op=ALU.add), r=qa.b + qb.b, w=qr.b)
                for c_ in range(8):
                    C('pe', lambda e, c_=c_: e.transpose(out=bkb(7)[:, c_ * 128:(c_ + 1) * 128],
                                                         in_=qr.ap[:, c_ * 128:(c_ + 1) * 128], identity=ident.ap),
                      r=qr.b + ident.b, w=[PB[7]])
                for par in range(2):
                    C('act', lambda e, qs=qs, par=par: e.activation(
                        out=qT.ap[:, qs, par], in_=bkb(7).rearrange("p (k t) -> p k t", k=8), func=AF.Identity,
                        scale=rowm.ap[:, par:par + 1]), r=[PB[7]] + rowm.b, w=[qT.b[qs]])

            def att_A(t, g, ps):
                qs = t % 2
                for kb in range(5):
                    sb_ = 2 + sbi[0] % 2
                    sbi[0] += 1
                    for h in range(4):
                        c_ = 2 * g + h // 2
                        if kb < 3:
                            kl = kT.ap[:, g, (t + kb) * 128:(t + kb + 1) * 128]
                            kbuf = [kT.b[t + kb]]
                        else:
                            kl = kcT.ap[:, g, (kb - 3) * 128:(kb - 2) * 128]
                            kbuf = [kcT.b[kb - 3]]
                        C('pe', mmf(bk(sb_)[:, h * 128:(h + 1) * 128], kl, qT.ap[:, qs, h % 2, c_, :]),
                          r=kbuf + [qT.b[qs]], w=[PB[sb_]])
                    C('act', lambda e, sb_=sb_, ps=ps, kb=kb: e.activation(
                        out=PT.ap[:, ps, kb, :], in_=bk(sb_), func=AF.Exp, scale=0.125), r=[PB[sb_]], w=[PT.b[ps]])
                    if kb in (0, 2):
                        if kb == 0:
                            mi = 2 if (hf == 0 and t == 0) else 0
                        else:
                            mi = 3 if (hf == 1 and t == NT - 1) else 1
                        C('dve', lambda e, ps=ps, kb=kb, mi=mi: e.tensor_tensor(
                            out=PT.ap[:, ps, kb, :], in0=PT.ap[:, ps, kb, :], in1=masks.ap[:, mi, :], op=ALU.mult),
                          r=[PT.b[ps]] + masks.b, w=[PT.b[ps]])

            def att_B(t, g, ps, ub):
                for kb in range(5):
                    if kb < 3:
                        vl = vd.ap[:, t + kb, g].rearrange("p a d -> p (a d)")
                        vbuf = [vd.b[t + kb]]
                    else:
                        vl = vcd.ap[:, kb - 3, g].rearrange("p a d -> p (a d)")
                        vbuf = [vcd.b[kb - 3]]
                    C('pe', mmf(bk(ub), vl, PT.ap[:, ps, kb, :], kb == 0, kb == 4), r=vbuf + [PT.b[ps]], w=[PB[ub]])
                for kb in range(5):
                    C('pe', mmf(bk(6), onesb.ap, PT.ap[:, ps, kb, :], kb == 0, kb == 4),
                      r=onesb.b + [PT.b[ps]], w=[PB[6]])
                C('dve', lambda e, g=g: e.tensor_tensor(
                    out=rden.ap, in0=bk(6), in1=eskB.ap[:, 4 * g:4 * g + 4, :].rearrange("p h q -> p (h q)"),
                    op=ALU.add), r=[PB[6]] + eskB.b, w=rden.b)
                C('dve', lambda e: e.reciprocal(out=rden.ap, in_=rden.ap), r=rden.b, w=rden.b)
                C('dve', lambda e: e.tensor_tensor(out=rden.ap, in0=rden.ap, in1=m4.ap, op=ALU.mult),
                  r=rden.b + m4.b, w=rden.b)
                C('dve', lambda e, ub=ub: e.tensor_tensor(out=prod.ap, in0=bk(ub), in1=rden.ap, op=ALU.mult),
                  r=[PB[ub]] + rden.b, w=prod.b)
                p4 = prod.ap.rearrange("p (c par q) -> p c par q", c=2, par=2)
                C('dve', lambda e, p4=p4, g=g, t=t: e.tensor_tensor(
                    out=attnT.ap[:, 2 * g:2 * g + 2, t * 128:(t + 1) * 128], in0=p4[:, :, 0, :], in1=p4[:, :, 1, :],
                    op=ALU.add), r=prod.b, w=[attnT.b[t]])

            groups = [(t, g) for t in range(NT) for g in range(4)]
            for idx in range(len(groups) + 1):
                if idx < len(groups):
                    t, g = groups[idx]
                    if g == 0:
                        att_prologue(t)
                    att_A(t, g, idx % 3)
                if idx >= 1:
                    t_, g_ = groups[idx - 1]
                    att_B(t_, g_, (idx - 1) % 3, 4 + (idx - 1) % 2)
            phase_end('att', hf, {'attnT': ((attnT.ap, attnT.b), [128, 8, HALF]), 'poolT': ((poolT.ap, poolT.b), [128, 8, HALF]), 'kT': ((kT.ap, kT.b), [128, 4, NTH * 128]), 'vd': ((vd.ap, vd.b), [128, NTH, 4, 2, 64]), 'kcT': ((kcT.ap, kcT.b), [128, 4, 256])})
            ar.release(wq, qf, qa, qb, qr, qT, PT, rden, kT, vd, kcT, vcd, masks, rc, rs_, bqkv, esk, eskB, rowm, m4, prod)

            mT = A("mT", [8, HALF], BF16, nb=NT, top=True)
            wga = A("wga", [8, D], BF16)
            wgp = A("wgp", [8, D], BF16)
            wba = A("wba", [8, D], BF16)
            wbp = A("wbp", [8, D], BF16)
            load(wga, w_in[:, 2560:3584].rearrange("(k p) n -> p k n", p=128), cast=True)
            load(wba, w_abr.rearrange("(k p) n -> p k n", p=128), cast=True)
            load(wgp, w_in[:, 3584:4608].rearrange("(k p) n -> p k n", p=128), cast=True)
            load(wbp, w_pbr.rearrange("(k p) n -> p k n", p=128), cast=True)
            sga = A("sga", [2, 512], F32, nb=2)
            sgp = A("sgp", [2, 512], F32, nb=2)
            t1 = A("t1", [2, 512], F32, nb=2)
            t2 = A("t2", [2, 512], F32, nb=2)
            it = 0
            for tc in range(2):
                hcols = slice(128 + tc * 512, 128 + (tc + 1) * 512)
                hbufs = [hT.b[i] for i in tiles_of(128 + tc * 512, 128 + (tc + 1) * 512)]
                tcs = slice(tc * 512, (tc + 1) * 512)
                tbufs = list(range(tc * 4, tc * 4 + 4))
                for m in range(8):
                    s = it % 2
                    it += 1
                    b0 = 4 * s
                    ms = slice(m * 128, (m + 1) * 128)
                    for k in range(8):
                        C('pe', mmf(bk(b0), wga.ap[:, k, ms], hT.ap[:, k, hcols], k == 0, k == 7),
                          r=wga.b + hbufs, w=[PB[b0]])
                    C('act', lambda e, b0=b0, s=s, m=m: e.activation(out=sga.ap[:, s], in_=bk(b0), func=AF.Sigmoid,
                                                                     bias=bcols.ap[:, 8 + m:9 + m]),
                      r=[PB[b0]] + bcols.b, w=[sga.b[s]])
                    for k in range(8):
                        C('pe', mmf(bk(b0 + 1), wgp.ap[:, k, ms], hT.ap[:, k, hcols], k == 0, k == 7),
                          r=wgp.b + hbufs, w=[PB[b0 + 1]])
                    C('act', lambda e, b0=b0, s=s, m=m: e.activation(out=sgp.ap[:, s], in_=bk(b0 + 1), func=AF.Sigmoid,
                                                                     bias=bcols.ap[:, 16 + m:17 + m]),
                      r=[PB[b0 + 1]] + bcols.b, w=[sgp.b[s]])
                    for k in range(8):
                        C('pe', mmf(bk(b0 + 2), wba.ap[:, k, ms], attnT.ap[:, k, tcs], k == 0, k == 7),
                          r=wba.b + [attnT.b[j] for j in tbufs], w=[PB[b0 + 2]])
                    for k in range(8):
                        C('pe', mmf(bk(b0 + 3), wbp.ap[:, k, ms], poolT.ap[:, k, tcs], k == 0, k == 7),
                          r=wbp.b + poolT.b, w=[PB[b0 + 3]])
                    C('dve', lambda e, b0=b0, s=s: e.tensor_tensor(out=t1.ap[:, s], in0=bk(b0 + 2), in1=sga.ap[:, s],
                                                                   op=ALU.mult), r=[PB[b0 + 2], sga.b[s]], w=[t1.b[s]])
                    C('dve', lambda e, b0=b0, s=s: e.tensor_tensor(out=t2.ap[:, s], in0=bk(b0 + 3), in1=sgp.ap[:, s],
                                                                   op=ALU.mult), r=[PB[b0 + 3], sgp.b[s]], w=[t2.b[s]])
                    C('dve', lambda e, s=s, m=m, tcs=tcs: e.tensor_tensor(out=mT.ap[:, m, tcs], in0=t1.ap[:, s],
                                                                         in1=t2.ap[:, s], op=ALU.add),
                      r=[t1.b[s], t2.b[s]], w=[mT.b[j] for j in tbufs])
            phase_end('br', hf, {'mT': ((mT.ap, mT.b), [128, 8, HALF])})
            ar.release(wga, wgp, wba, wbp, sga, sgp, t1, t2, attnT, poolT, hT)

            G1b = load_mod("G1b", 0, 2)
            A2b = load_mod("A2b", 0, 4)
            sh2b = load_mod("sh2b", 0, 3)
            wo = A("wo", [8, D], BF16)
            load(wo, w_outd.rearrange("(k p) n -> p k n", p=128), cast=True)
            h2T = A("h2T", [8, HALF], BF16, nb=NT, top=True)
            Gt = A("G", [NT, NE], F32, nb=NT, top=True)
            Gs = A("Gs", [NT, NE], F32, nb=NT, top=True)
            x1o = A("x1o", [2, D], F32, nb=2)
            GT = A("GT", [NT, 128], BF16, nb=NT, top=True)
            xs = A("xs", [2, D], F32, nb=2)
            tf = A("tf", [2, D], F32, nb=2)
            tb = A("tb", [2, D], BF16, nb=2)
            st2 = A("st2", [NT, 2], F32, nb=NT)
            rt = A("rt", [2, 4, NE], F32, nb=2)
            r8 = A("r8", [2, 16], F32, nb=2)
            gbf = A("gbf", [2, 128], BF16, nb=2)
            C('pool', lambda e, gbf=gbf: e.memset(gbf.ap, 0.0), w=gbf.b)
            for t in range(NT):
                s = t % 2
                row0 = 128 + hbase + t * 128
                orow = hbase + t * 128
                DMA('sp', xs.ap[:, s], xh[row0:row0 + 128, :], "L_%s_%d" % (xs.name, s), w=[xs.b[s]])
                for hh in range(2):
                    pb = 2 * s + hh
                    hs = slice(hh * 512, (hh + 1) * 512)
                    for k in range(8):
                        C('pe', mmf(bk(pb), mT.ap[:, k, t * 128:(t + 1) * 128], wo.ap[:, k, hs], k == 0, k == 7),
                          r=[mT.b[t]] + wo.b, w=[PB[pb]])
                    C('dve', lambda e, pb=pb, s=s, hs=hs: e.tensor_tensor(out=tf.ap[:, s, hs], in0=bk(pb), in1=G1b.ap[:, hs],
                                                                         op=ALU.mult), r=[PB[pb]] + G1b.b, w=[tf.b[s]])
                    C('dve', lambda e, s=s, hs=hs: e.tensor_tensor(out=x1o.ap[:, s, hs], in0=tf.ap[:, s, hs],
                                                                   in1=xs.ap[:, s, hs], op=ALU.add),
                      r=[tf.b[s], xs.b[s]], w=[x1o.b[s]])
                DMA('sp', outd[orow:orow + 128, :], x1o.ap[:, s], "S_x1", r=[x1o.b[s]], w=[x1B[hf * 8 + t]])
                norm_mod_T(x1o.ap[:, s], [x1o.b[s]], A2b, sh2b, st2, t, (tf.ap[:, s], tf.b[s]), (tb.ap[:, s], tb.b[s]),
                           6 + s, h2T.ap[:, :, t * 128:(t + 1) * 128], [h2T.b[t]])
                for k in range(8):
                    C('pe', mmf(bk(4)[:, 0:NE], h2T.ap[:, k, t * 128:(t + 1) * 128], wr.ap[:, k, :], k == 0, k == 7),
                      r=[h2T.b[t]] + wr.b, w=[PB[4]])
                lg, mk, ex, exm = (rt.ap[:, s, j, :] for j in range(4))
                rb_ = [rt.b[s]]
                r8b = [r8.b[s]]
                top8 = r8.ap[:, s, 0:8]
                C('dve', lambda e, lg=lg: e.tensor_tensor(out=lg, in0=bk(4)[:, 0:NE], in1=brt.ap, op=ALU.add),
                  r=[PB[4]] + brt.b, w=rb_)
                C('dve', lambda e, lg=lg, top8=top8: e.max(out=top8, in_=lg), r=rb_, w=r8b)
                C('dve', lambda e, lg=lg, mk=mk, s=s: e.tensor_scalar(out=mk, in0=lg, scalar1=r8.ap[:, s, 3:4], scalar2=None,
                                                                     op0=ALU.is_ge), r=rb_ + r8b, w=rb_)
                C('dve', lambda e, s=s: e.tensor_scalar(out=r8.ap[:, s, 8:9], in0=r8.ap[:, s, 0:1], scalar1=-1.0,
                                                        scalar2=None, op0=ALU.mult), r=r8b, w=r8b)
                C('act', lambda e, lg=lg, ex=ex, s=s: e.activation(out=ex, in_=lg, func=AF.Exp, bias=r8.ap[:, s, 8:9]),
                  r=rb_ + r8b, w=rb_)
                C('dve', lambda e, ex=ex, mk=mk, exm=exm: e.tensor_tensor(out=exm, in0=ex, in1=mk, op=ALU.mult), r=rb_, w=rb_)
                C('dve', lambda e, exm=exm, s=s: e.reduce_sum(out=r8.ap[:, s, 9:10], in_=exm, axis=AX.X), r=rb_, w=r8b)
                C('dve', lambda e, s=s: e.reciprocal(out=r8.ap[:, s, 10:11], in_=r8.ap[:, s, 9:10]), r=r8b, w=r8b)
                C('dve', lambda e, exm=exm, s=s, t=t: e.tensor_scalar(out=Gt.ap[:, t, :], in0=exm, scalar1=r8.ap[:, s, 10:11],
                                                                     scalar2=None, op0=ALU.mult), r=rb_ + r8b, w=[Gt.b[t]])
                C('dve', lambda e, s=s, t=t: e.tensor_copy(out=gbf.ap[:, s, 0:NE], in_=Gt.ap[:, t, :]), r=[Gt.b[t]], w=[gbf.b[s]])
                C('dve', lambda e, t=t: e.tensor_scalar(out=Gs.ap[:, t, :], in0=Gt.ap[:, t, :], scalar1=1.0 / 1.702,
                                                        scalar2=None, op0=ALU.mult), r=[Gt.b[t]], w=[Gs.b[t]])
                C('pe', lambda e, s=s: e.transpose(out=bkb(5)[:, 0:128], in_=gbf.ap[:, s], identity=ident.ap),
                  r=[gbf.b[s]] + ident.b, w=[PB[5]])
                C('act', lambda e, t=t: e.activation(out=GT.ap[:, t, :], in_=bkb(5)[:, 0:128], func=AF.Copy),
                  r=[PB[5]], w=[GT.b[t]])
            phase_end('out', hf, {'h2T': ((h2T.ap, h2T.b), [128, 8, HALF]), 'G': ((Gt.ap, Gt.b), [128, NT, NE]), 'GT': ((GT.ap, GT.b), [128, NT, 128]), 'gbf': ((gbf.ap, gbf.b), [128, 2, 128]), 'bdn': ((bdn.ap, bdn.b), [128, D])})
            ar.release(G1b, A2b, sh2b, wo, xs, x1o, tf, tb, st2, rt, r8, gbf, mT)

            acc = A("acc", [NT, D], F32, nb=NT, top=True)
            wgu = A("wgu", [2, 4, 2, 2 * D], BF16, nb=8)
            wdn = A("wdn", [1, 2, 4, D], BF16, nb=2)
            for t in range(NT):
                for hh in range(2):
                    pb = 4 + (t * 2 + hh) % 4
                    hs = slice(hh * 512, (hh + 1) * 512)
                    C('pe', mmf(bk(pb), GT.ap[:, t, :], bdn.ap[:, hs]), r=[GT.b[t]] + bdn.b, w=[PB[pb]])
                    C('act', lambda e, pb=pb, t=t, hs=hs: e.activation(out=acc.ap[:, t, hs], in_=bk(pb), func=AF.Copy),
                      r=[PB[pb]], w=[acc.b[t]])
            actT = A("actT", [2, 8, 512], BF16, nb=2)
            gc = A("gc", [2, 512], F32, nb=2)
            sg = A("sg", [2, 512], F32, nb=2)
            lc = A("lc", [2, 512], F32, nb=2)
            yi = 0
            pi = 0
            for e_ in range(NEXP):
                es = e_ % 2
                gsrc = w_gu[e_].rearrange("(k p) n -> p k n", p=128)
                dsrc = w_down[e_].rearrange("(k p) n -> p k n", p=128)
                for j in range(4):
                    DMA('pool', wgu.ap[:, es, j], gsrc[:, 2 * j:2 * j + 2, :], "L_%s_%d_%d" % (wgu.name, es, j), w=[wgu.b[es * 4 + j]])
                for j in range(2):
                    DMA('pool', wdn.ap[:, 0, j], dsrc[:, 4 * j:4 * j + 4, :], "L_%s_%d_%d" % (wdn.name, es, j), w=[wdn.b[j]])
                for tc in range(2):
                    as_ = (e_ * 2 + tc) % 2
                    tcs = slice(tc * 512, (tc + 1) * 512)
                    hb_ = [h2T.b[j] for j in range(tc * 4, tc * 4 + 4)]
                    for mp in range(8):
                        s = pi % 2
                        pi += 1
                        bg, bl = 2 * s, 2 * s + 1
                        for k in range(8):
                            C('pe', mmf(bk(bg), wgu.ap[:, es, k // 2, k % 2, mp * 128:(mp + 1) * 128], h2T.ap[:, k, tcs],
                                        k == 0, k == 7), r=[wgu.b[es * 4 + k // 2]] + hb_, w=[PB[bg]])
                        for k in range(8):
                            C('pe', mmf(bk(bl), wgu.ap[:, es, k // 2, k % 2, D + mp * 128:D + (mp + 1) * 128],
                                        h2T.ap[:, k, tcs], k == 0, k == 7), r=[wgu.b[es * 4 + k // 2]] + hb_, w=[PB[bl]])
                        C('dve', lambda e, bg=bg, s=s, e_=e_, mp=mp: e.tensor_scalar(
                            out=gc.ap[:, s], in0=bk(bg), scalar1=bguc.ap[:, e_, mp:mp + 1], scalar2=7.0,
                            op0=ALU.add, op1=ALU.min), r=[PB[bg]] + bguc.b, w=[gc.b[s]])
                        C('act', lambda e, s=s: e.activation(out=sg.ap[:, s], in_=gc.ap[:, s], func=AF.Silu, scale=1.702),
                          r=[gc.b[s]], w=[sg.b[s]])
                        C('dve', lambda e, bl=bl, s=s, e_=e_, mp=mp: e.tensor_scalar(
                            out=lc.ap[:, s], in0=bk(bl), scalar1=bguc.ap[:, e_, 8 + mp:9 + mp], scalar2=8.0,
                            op0=ALU.add, op1=ALU.min), r=[PB[bl]] + bguc.b, w=[lc.b[s]])
                        C('dve', lambda e, s=s, as_=as_, mp=mp: e.scalar_tensor_tensor(
                            out=actT.ap[:, as_, mp, :], in0=lc.ap[:, s], scalar=-6.0, in1=sg.ap[:, s],
                            op0=ALU.max, op1=ALU.mult), r=[lc.b[s], sg.b[s]], w=[actT.b[as_]])
                    for tt in range(4):
                        t = tc * 4 + tt
                        for hh in range(2):
                            pb = 4 + yi % 4
                            yi += 1
                            hs = slice(hh * 512, (hh + 1) * 512)
                            for k in range(8):
                                C('pe', mmf(bk(pb), actT.ap[:, as_, k, tt * 128:(tt + 1) * 128],
                                            wdn.ap[:, 0, k // 4, k % 4, hs], k == 0, k == 7),
                                  r=[actT.b[as_], wdn.b[k // 4]], w=[PB[pb]])
                            C('dve', lambda e, pb=pb, t=t, hs=hs, e_=e_: e.scalar_tensor_tensor(
                                out=acc.ap[:, t, hs], in0=bk(pb), scalar=Gs.ap[:, t, e_:e_ + 1], in1=acc.ap[:, t, hs],
                                op0=ALU.mult, op1=ALU.add), r=[PB[pb], Gs.b[t], acc.b[t]], w=[acc.b[t]])
            phase_end('moe', hf, {'acc': ((acc.ap, acc.b), [128, NT, D])})
            ar.release(wgu, wdn, actT, gc, sg, lc, h2T, Gt, Gs, GT)

            G2b = load_mod("G2b", 0, 5)
            fgb = A("fgb", [D], F32)
            load(fgb, fg.partition_broadcast(128))
            xr = A("xr", [2, D], F32, nb=2)
            xo = A("xo", [2, D], F32, nb=2)
            tfo = A("tfo", [2, D], F32, nb=2)
            st3 = A("st3", [NT, 2], F32, nb=NT)
            for t in range(NT):
                s = t % 2
                orow = hbase + t * 128
                DMA('sp', xr.ap[:, s], outd[orow:orow + 128, :], "L_xr", r=[x1B[hf * 8 + t]], w=[xr.b[s]])
                C('dve', lambda e, s=s, t=t, acc=acc, G2b=G2b, tfo=tfo: e.tensor_tensor(
                    out=tfo.ap[:, s], in0=acc.ap[:, t, :], in1=G2b.ap, op=ALU.mult), r=[acc.b[t]] + G2b.b, w=[tfo.b[s]])
                C('dve', lambda e, s=s, xo=xo, tfo=tfo, xr=xr: e.tensor_tensor(
                    out=xo.ap[:, s], in0=tfo.ap[:, s], in1=xr.ap[:, s], op=ALU.add), r=[tfo.b[s], xr.b[s]], w=[xo.b[s]])
                norm_mod_T(xo.ap[:, s], [xo.b[s]], fgb, None, st3, t, (tfo.ap[:, s], tfo.b[s]), None, None, None, None)
                final_ops.append(DMA('sp', outd[orow:orow + 128, :], tfo.ap[:, s], "S_out", r=[tfo.b[s]], w=[x1B[hf * 8 + t]]))
            phase_end('fin', hf, {'tfin': ((tfo.ap, tfo.b), [128, 2, D]), 'accf': ((acc.ap, acc.b), [128, NT, D])})
            ar.release(G2b, fgb, xr, xo, tfo, st3, acc)

    except _Stop:
        pass
    P.emit(nc, final_ops=final_ops)
    return nc


def build_program_safe(stop=None, dumps=None):
    return build_program(stop, dumps)


def _host_consts(j):
    L = 8192
    ident = np.eye(128, dtype=np.float32)
    jj = np.arange(128)[:, None]
    ii = np.arange(128)[None, :]
    mP = (jj >= ii).astype(np.float32)
    mN = (jj <= ii).astype(np.float32)
    mPF = mP if j != 0 else np.zeros_like(mP)
    mNL = mN if j != 3 else np.zeros_like(mN)
    masks = np.concatenate([np.tile(m, (1, 4)) for m in (mP, mN, mPF, mNL)], axis=1).astype(np.float32)
    pos = j * 2048 - 128 + np.arange(2304)
    posc = np.clip(pos, 0, L - 1)
    row = (posc // 64).astype(np.float32)
    col = (posc % 64).astype(np.float32)
    inv_freq = (np.float32(10000.0) ** (-np.arange(16, dtype=np.float32) / np.float32(16))).astype(np.float32)
    ang = np.stack([row[:, None] * inv_freq, col[:, None] * inv_freq], axis=1).astype(np.float32)
    cs, sn = np.cos(ang).astype(np.float32), np.sin(ang).astype(np.float32)
    cos64 = np.stack([cs, cs], axis=2).reshape(2304, 64)
    sin64 = np.stack([-sn, sn], axis=2).reshape(2304, 64)
    ropec = cos64.reshape(18, 128, 64).transpose(1, 0, 2).reshape(128, 18 * 64)
    ropes = sin64.reshape(18, 128, 64).transpose(1, 0, 2).reshape(128, 18 * 64)
    pcorr = np.ones((2, 4, 16), np.float32)
    uval = np.ones((2, 2), np.float32)
    for hf in range(2):
        p0 = j * 2048 + hf * 1024
        for g in range(4):
            h = 2 ** g
            w = 2 * h
            for side, ts in ((0, range(0, 8)), (1, range(1016, 1024))):
                for q, t in enumerate(ts):
                    p = p0 + t
                    cnt = min(p + h, L) - max(p - h, 0)
                    pcorr[hf, g, side * 8 + q] = w / cnt
        if p0 == 0:
            uval[hf, 0] = 0.0
        if p0 + 1024 == L:
            uval[hf, 1] = 0.0
    rowmask = np.zeros((128, 2), np.float32)
    rowmask[:64, 0] = 1.0
    rowmask[64:, 1] = 1.0
    m4 = np.zeros((128, 4, 128), np.float32)
    for h in range(4):
        m4[:, h, :] = rowmask[:, h % 2][:, None]
    return dict(rowmask=rowmask, m4=m4.reshape(128, 512), ident=ident, masks=masks, ropec=np.ascontiguousarray(ropec), ropes=np.ascontiguousarray(ropes),
                pcorr=pcorr.reshape(1, -1), uval=uval.reshape(1, -1))


def make_in_maps(x, c, ctx, c_ctx, w_ada, b_ada, norm1_g, norm2_g, w_in, b_in, attn_sink, w_pool, pool_scale,
                 w_attn_br, w_pool_br, w_out, w_router, b_router, w_gu, b_gu, w_down, b_down, final_g):
    f = lambda a: np.ascontiguousarray(np.asarray(a, dtype=np.float32))
    x, c, ctx, c_ctx = f(x), f(c), f(ctx), f(c_ctx)
    shared = dict(
        w_ada=f(w_ada[0]), b_ada=f(b_ada[0]).reshape(1, -1), n1g=f(norm1_g[0]).reshape(1, -1),
        n2g=f(norm2_g[0]).reshape(1, -1), fg=f(final_g).reshape(1, -1), w_in=f(w_in[0]),
        b_in=f(b_in[0]).reshape(1, -1), b_in_cols=f(np.asarray(b_in[0])[1536:].reshape(24, 128).T),
        sink=f(attn_sink[0]).reshape(1, -1), w_pool=f(w_pool[0]),
        pscale_cols=f(np.asarray(pool_scale[0]).reshape(8, 128).T), w_abr=f(w_attn_br[0]), w_pbr=f(w_pool_br[0]),
        w_out=f(w_out[0]), w_router=f(w_router[0]), b_router=f(b_router[0]).reshape(1, -1), w_gu=f(w_gu[0]),
        b_gu_cols=f(np.asarray(b_gu[0]).reshape(NE, 16, 128).transpose(2, 0, 1).reshape(128, NE * 16)),
        w_down=f(w_down[0]), b_down=f(b_down[0]))
    maps = []
    for r in range(8):
        b, j = r // 4, r % 4
        xhalo = np.zeros((2304, D), np.float32)
        lo, hi = j * 2048 - 128, (j + 1) * 2048 + 128
        slo, shi = max(lo, 0), min(hi, 8192)
        xhalo[slo - lo: shi - lo] = x[b, slo:shi]
        ccols = np.stack([c[b].reshape(8, 128).T, c_ctx.reshape(8, 128).T], axis=2).reshape(128, 16)
        m = dict(shared)
        m.update(xh=xhalo, ctx=f(ctx[b]), ccols=f(ccols))
        m.update(_host_consts(j))
        maps.append(m)
    return maps


_NC_CACHE = {}


def kernel(**inputs):
    if "nc" not in _NC_CACHE:
        _NC_CACHE["nc"] = build_program()
    nc = _NC_CACHE["nc"]
    maps = make_in_maps(**inputs)
    res = run_bass_kernel_spmd(nc, maps, core_ids=list(range(8)))
    out = np.stack([r["out"] for r in res.results], axis=0)
    return out.reshape(2, 4, 2048, D).reshape(2, 8192, D).astype(np.float32)
```

```python
import contextlib
import numpy as np
import concourse.bass as bass
import concourse.mybir as mybir
from concourse.bass_utils import run_bass_kernel_spmd

F32 = mybir.dt.float32
BF16 = mybir.dt.bfloat16
AF = mybir.ActivationFunctionType
ALU = mybir.AluOpType
AX = mybir.AxisListType

D = 1024
NE = 32
HALF = 1024
NT = 8
NTH = 10
DEBUG = False
import os
NEXP = int(os.environ.get('NEXP', '32'))


class Buf:
    __slots__ = ("name", "lastw", "readers")

    def __init__(self, name):
        self.name = name
        self.lastw = None
        self.readers = []


class Op:
    __slots__ = ("eng", "fn", "deps", "kind", "dsem", "ndep", "tok")

    def __init__(self, eng, fn, kind, dsem=None):
        self.eng = eng
        self.fn = fn
        self.kind = kind
        self.dsem = dsem
        self.deps = []
        self.ndep = 0
        self.tok = None


class Prog:
    ENGS = ("pe", "act", "dve", "pool", "sp")

    def __init__(self):
        self.ops = {e: [] for e in self.ENGS}
        self.dma_sems = []

    def _add(self, op, reads, writes):
        deps = []
        for b in reads:
            w = b.lastw
            if w is not None:
                deps.append(w)
        for b in writes:
            w = b.lastw
            if w is not None:
                if w.kind == 'c' and op.kind == 'c' and w.eng == op.eng:
                    pass
                else:
                    deps.append(w)
            for r in b.readers:
                if r.kind == 'c' and op.kind == 'c' and r.eng == op.eng:
                    continue
                deps.append(r)
        seen = set()
        for d in deps:
            if d is op or id(d) in seen:
                continue
            seen.add(id(d))
            if d.kind == 'c' and op.kind == 'c' and d.eng == 'pe' and op.eng == 'pe':
                continue
            op.deps.append(d)
            d.ndep += 1
        for b in writes:
            b.lastw = op
            b.readers = []
        for b in reads:
            if b.lastw is not op:
                b.readers.append(op)
        self.ops[op.eng].append(op)
        return op

    def c(self, eng, fn, reads=(), writes=()):
        return self._add(Op(eng, fn, 'c'), reads, writes)

    def dma(self, eng, fn, dsem, reads=(), writes=()):
        if dsem not in self.dma_sems:
            self.dma_sems.append(dsem)
        return self._add(Op(eng, fn, 'd', dsem), reads, writes)

    def emit(self, nc, final_ops=()):
        CH = 8000
        NPOOL = 16
        with contextlib.ExitStack() as st:
            esems = {}
            sems = {}
            for e in self.ENGS:
                n = 0
                nd = 0
                for op in self.ops[e]:
                    if op.kind == 'd':
                        key = ("dma", e, nd % NPOOL)
                        nd += 1
                        if key not in sems:
                            sems[key] = [st.enter_context(nc.semaphore("d_%s%d" % (e, key[2]))), 0]
                        op.dsem = (key, sems[key][1])
                        sems[key][1] += 16
                        op.tok = (key, sems[key][1])
                    elif op.ndep > 0:
                        n += 1
                        key = (e, (n - 1) // CH)
                        if key not in sems:
                            sems[key] = [st.enter_context(nc.semaphore("e_%s%d" % key)), 0]
                        op.tok = (key, (n - 1) % CH + 1)
            block = st.enter_context(nc.Block())

            def run(eng_name, eng):
                waited = {}
                for op in self.ops[eng_name]:
                    need = {}
                    for d in op.deps:
                        k, v = d.tok
                        if waited.get(k, 0) < v and need.get(k, 0) < v:
                            need[k] = v
                    if op.kind == 'd':
                        k, v = op.dsem
                        if v > 0 and waited.get(k, 0) < v and need.get(k, 0) < v:
                            need[k] = v
                    for k, v in need.items():
                        waited[k] = v
                        eng.wait_ge(sems[k][0], v)
                    ins = op.fn(eng)
                    if op.kind == 'd':
                        ins.then_inc(sems[op.tok[0]][0], 16)
                    elif op.tok is not None:
                        ins.then_inc(sems[op.tok[0]][0], 1)
                if eng_name == 'sp':
                    for d in final_ops:
                        k, v = d.tok
                        eng.wait_ge(sems[k][0], v)

            block.tensor(lambda eng: run('pe', eng))
            block.scalar(lambda eng: run('act', eng))
            block.vector(lambda eng: run('dve', eng))
            block.gpsimd(lambda eng: run('pool', eng))
            block.sync(lambda eng: run('sp', eng))
            self.nsems = len(sems)


class T:
    __slots__ = ("name", "ap", "b", "start", "end", "flat", "uid")


class Arena:
    def __init__(self, nc, nbytes):
        self.nbytes = nbytes
        self.t = nc.sbuf_tensor("arena", [128, nbytes // 2], BF16).__enter__()
        self.free = [(0, nbytes)]
        self.grave = []
        self.live = {}
        self.peak = 0
        self.uid = 0

    def alloc(self, name, shape, dt, nb=1, parts=128, top=False):
        n = 1
        for s in shape:
            n *= s
        nbytes = n * (4 if dt == F32 else 2)
        nbytes = (nbytes + 63) // 64 * 64
        order = range(len(self.free) - 1, -1, -1) if top else range(len(self.free))
        for i in order:
            s, e = self.free[i]
            if e - s >= nbytes:
                if top:
                    self.free[i] = (s, e - nbytes)
                    s = e - nbytes
                else:
                    self.free[i] = (s + nbytes, e)
                if self.free[i][0] == self.free[i][1]:
                    del self.free[i]
                break
        else:
            raise RuntimeError("arena OOM for %s (%d B); live=%s" % (name, nbytes, sorted(
                (v_.end - v_.start, k) for k, v_ in self.live.items())))
        t = T()
        self.uid += 1
        t.uid = self.uid
        t.name = "%s_%d" % (name, self.uid)
        t.start, t.end = s, s + nbytes
        self.peak = max(self.peak, t.end)
        v = self.t[0:parts, s // 2:(s + nbytes) // 2]
        if dt != BF16:
            v = v.bitcast(dt)
        v = v[:, 0:n]
        t.flat = v
        if len(shape) == 2:
            v = v.rearrange("p (a b) -> p a b", a=shape[0])
        elif len(shape) == 3:
            v = v.rearrange("p (a b c) -> p a b c", a=shape[0], b=shape[1])
        elif len(shape) == 4:
            v = v.rearrange("p (a b c d) -> p a b c d", a=shape[0], b=shape[1], c=shape[2])
        t.ap = v
        t.b = [Buf("%s.%d" % (name, i)) for i in range(nb)]
        inh = []
        for (gs, ge, bufs) in self.grave:
            if gs < t.end and ge > t.start:
                for gb in bufs:
                    if gb.lastw is not None:
                        inh.append(gb.lastw)
                    inh.extend(gb.readers)
        if inh:
            seen = set()
            u = []
            for o in inh:
                if id(o) not in seen:
                    seen.add(id(o))
                    u.append(o)
            for b in t.b:
                b.readers = list(u)
        self.live[t.name] = t
        return t

    def release(self, *ts):
        for t in ts:
            del self.live[t.name]
            self.grave.append((t.start, t.end, t.b))
            self.free.append((t.start, t.end))
        self.free.sort()
        m = []
        for s, e in self.free:
            if m and m[-1][1] == s:
                m[-1] = (m[-1][0], e)
            else:
                m.append((s, e))
        self.free = m


class _Stop(Exception):
    pass


def build_program(stop=None, dumps=None):
    nc = bass.Bass("TRN2", target_bir_lowering=False)
    dumps = {} if dumps is None else dumps

    def phase_end(name, hf, env):
        if stop is not None and stop == (name, hf):
            for dn, (fn_ap, shape) in env.items():
                dt_ = nc.dram_tensor("dbg_" + dn, list(shape), F32, kind="ExternalOutput").ap()
                t_, bufs_ = fn_ap
                final_ops.append(P.dma('pool', lambda e, dt_=dt_, t_=t_: e.dma_start(out=dt_, in_=t_), "dbg", reads=bufs_))
                dumps[dn] = shape
            raise _Stop()

    def din(name, shape):
        return nc.dram_tensor(name, list(shape), F32, kind="ExternalInput").ap()

    xh = din("xh", [2304, D])
    ctxd = din("ctx", [256, D])
    ccols = din("ccols", [128, 16])
    w_ada = din("w_ada", [D, 6 * D])
    b_ada = din("b_ada", [1, 6 * D])
    n1g = din("n1g", [1, D])
    n2g = din("n2g", [1, D])
    fg = din("fg", [1, D])
    w_in = din("w_in", [D, 4608])
    b_in = din("b_in", [1, 4608])
    b_in_cols = din("b_in_cols", [128, 24])
    sink = din("sink", [1, 16])
    w_pool = din("w_pool", [4, 256, 256])
    pscale_cols = din("pscale_cols", [128, 8])
    w_abr = din("w_abr", [D, D])
    w_pbr = din("w_pbr", [D, D])
    w_outd = din("w_out", [D, D])
    w_router = din("w_router", [D, NE])
    b_router = din("b_router", [1, NE])
    w_gu = din("w_gu", [NE, D, 2 * D])
    b_gu_cols = din("b_gu_cols", [128, NE * 16])
    w_down = din("w_down", [NE, D, D])
    b_down = din("b_down", [NE, D])
    identd = din("ident", [128, 128])
    masksd = din("masks", [128, 4 * 512])
    ropec = din("ropec", [128, 18 * 64])
    ropes = din("ropes", [128, 18 * 64])
    pcorr = din("pcorr", [1, 2 * 4 * 16])
    uval = din("uval", [1, 4])
    rowmaskd = din("rowmask", [128, 2])
    m4d = din("m4", [128, 512])
    outd = nc.dram_tensor("out", [2048, D], F32, kind="ExternalOutput").ap()
    modscr = nc.dram_tensor("modscr", [2, 6 * D], F32, kind="Internal").ap()
    dbg_out = {}

    P = Prog()
    ar = Arena(nc, 196608 - 16512 - 64)
    A = ar.alloc
    banks = [nc.psum_tensor("bank%d" % i, [128, 512], F32).__enter__() for i in range(8)]
    PB = [Buf("bank%d" % i) for i in range(8)]

    def bk(i):
        return banks[i][:]

    def bkb(i):
        return banks[i][:].bitcast(BF16)

    def C(eng, fn, r=(), w=()):
        return P.c(eng, fn, reads=r, writes=w)

    def mmf(out, lhsT, rhs, start=True, stop=True):
        return lambda e: e.matmul(out, lhsT=lhsT, rhs=rhs, start=start, stop=stop)

    def DMA(eng, out, in_, dsem, r=(), w=()):
        return P.dma(eng, lambda e: e.dma_start(out=out, in_=in_), dsem, reads=r, writes=w)

    def load(t, in_, cast=False, flat=False):
        return DMA('pool' if cast else 'sp', t.flat if flat else t.ap, in_, "L_" + t.name, w=t.b)

    final_ops = []
    modB = [[Buf("mod%d_%d" % (w, j)) for j in range(6)] for w in range(2)]
    x1B = [Buf("x1d%d" % i) for i in range(16)]

    try:
        ident = A("ident", [128], BF16)
        load(ident, identd[:, :], cast=True)
        onesb = A("onesb", [128], BF16)
        C('pool', lambda e: e.memset(onesb.ap, 1.0), w=onesb.b)
        onesf = A("onesf", [128], F32)
        C('pool', lambda e: e.memset(onesf.ap, 1.0), w=onesf.b)
        epst = A("eps", [1], F32)
        C('pool', lambda e: e.memset(epst.ap, 1e-5), w=epst.b)
        bcols = A("bcols", [24], F32)
        load(bcols, b_in_cols[:, :])
        pscol = A("pscol", [8], F32)
        load(pscol, pscale_cols[:, :])
        pcor = A("pcor", [2, 4, 16], F32)
        load(pcor, pcorr.partition_broadcast(128), flat=True)
        uv = A("uval", [4], F32)
        load(uv, uval.partition_broadcast(128))
        brt = A("brt", [NE], F32)
        load(brt, b_router.partition_broadcast(128))
        wr = A("wr", [8, NE], BF16)
        load(wr, w_router.rearrange("(k p) n -> p k n", p=128), cast=True)
        bguc = A("bguc", [NE, 16], F32)
        load(bguc, b_gu_cols.rearrange("p (a b) -> p a b", a=NE))
        C('dve', lambda e: e.tensor_scalar(out=bguc.ap[:, :, 8:16], in0=bguc.ap[:, :, 8:16], scalar1=1.0, scalar2=None,
                                           op0=ALU.add), r=bguc.b, w=bguc.b)
        bdn = A("bdn", [D], BF16)
        C('pool', lambda e: e.memset(bdn.ap, 0.0), w=bdn.b)
        DMA('pool', bdn.ap[0:NE, :], b_down[:, :], "L_bdn", w=bdn.b)
        cc = A("cc", [8, 2], F32)
        load(cc, ccols.rearrange("p (k w) -> p k w", w=2))
        scb = A("scb", [8, 2], BF16)
        C('act', lambda e: e.activation(out=scb.ap, in_=cc.ap, func=AF.Silu), r=cc.b, w=scb.b)
        badb = A("badb", [6 * D], F32, parts=2)
        load(badb, b_ada.partition_broadcast(2))
        ng = A("ng", [2, D], F32, parts=2)
        DMA('sp', ng.ap[:, 0, :], n1g.partition_broadcast(2), "L_ng0", w=ng.b)
        DMA('sp', ng.ap[:, 1, :], n2g.partition_broadcast(2), "L_ng1", w=ng.b)
        wad = A("wad", [2, 8, D], BF16, nb=2)
        mrow = A("mrow", [2, D], F32, nb=2, parts=2)
        for j in range(6):
            s = j % 2
            DMA('pool', wad.ap[:, s], w_ada[:, j * D:(j + 1) * D].rearrange("(k p) n -> p k n", p=128),
                "L_%s_%d" % (wad.name, s), w=[wad.b[s]])
            for hh in range(2):
                for k in range(8):
                    C('pe', mmf(banks[hh][0:2, :], scb.ap[:, k, :], wad.ap[:, s, k, hh * 512:(hh + 1) * 512],
                                k == 0, k == 7), r=[wad.b[s]] + scb.b, w=[PB[hh]])
                C('dve', lambda e, hh=hh, s=s, j=j: e.tensor_tensor(
                    out=mrow.ap[:, s, hh * 512:(hh + 1) * 512], in0=banks[hh][0:2, :],
                    in1=badb.ap[:, j * D + hh * 512: j * D + (hh + 1) * 512], op=ALU.add),
                  r=[PB[hh]] + badb.b, w=[mrow.b[s]])
            if j in (1, 4):
                gi = 0 if j == 1 else 1
                C('dve', lambda e, s=s, gi=gi: e.scalar_tensor_tensor(
                    out=mrow.ap[:, s, :], in0=mrow.ap[:, s, :], scalar=1.0, in1=ng.ap[:, gi, :],
                    op0=ALU.add, op1=ALU.mult), r=[mrow.b[s]] + ng.b, w=[mrow.b[s]])
            DMA('sp', modscr[:, j * D:(j + 1) * D], mrow.ap[:, s, :], "S_%s_%d" % (mrow.name, s), r=[mrow.b[s]],
                w=[modB[0][j], modB[1][j]])
        ar.release(cc, scb, badb, ng, wad, mrow)

        def load_mod(name, wsel, j):
            t = A(name, [D], F32)
            DMA('sp', t.ap, modscr[wsel:wsel + 1, j * D:(j + 1) * D].partition_broadcast(128), "L_" + t.name,
                r=[modB[wsel][j]], w=t.b)
            return t

        def norm_mod_T(src_ap, src_bufs, Ab, shb, stat, si, tmpf, tmpb, trb, dstT_ap, dst_bufs):
            ssq = stat.ap[:, si, 0:1]
            rstd = stat.ap[:, si, 1:2]
            sb = [stat.b[si]]
            C('act', lambda e: e.activation(out=tmpf[0], in_=src_ap, func=AF.Square, accum_out=ssq),
              r=src_bufs, w=[tmpf[1]] + sb)
            C('act', lambda e: e.activation(out=rstd, in_=ssq, func=AF.Sqrt, scale=1.0 / D, bias=epst.ap[:, 0:1]),
              r=sb + epst.b, w=sb)
            C('dve', lambda e: e.reciprocal(out=rstd, in_=rstd), r=sb, w=sb)
            C('dve', lambda e: e.scalar_tensor_tensor(out=tmpf[0], in0=src_ap, scalar=rstd, in1=Ab.ap,
                                                      op0=ALU.mult, op1=ALU.mult),
              r=list(src_bufs) + sb + Ab.b, w=[tmpf[1]])
            if shb is not None:
                C('dve', lambda e: e.tensor_tensor(out=tmpb[0], in0=tmpf[0], in1=shb.ap, op=ALU.add),
                  r=[tmpf[1]] + shb.b, w=[tmpb[1]])
            else:
                return
            for k in range(8):
                C('pe', lambda e, k=k: e.transpose(out=bkb(trb)[:, k * 128:(k + 1) * 128],
                                                   in_=tmpb[0][:, k * 128:(k + 1) * 128], identity=ident.ap),
                  r=[tmpb[1]] + ident.b, w=[PB[trb]])
            C('act', lambda e: e.activation(out=dstT_ap, in_=bkb(trb).rearrange("p (k t) -> p k t", k=8), func=AF.Copy),
              r=[PB[trb]], w=dst_bufs)

        def tiles_of(a, b):
            return list(range(a // 128, (b - 1) // 128 + 1))

        for hf in range(2):
            hbase = hf * HALF
            A1b = load_mod("A1b", 0, 1)
            sh1b = load_mod("sh1b", 0, 0)
            A1cb = load_mod("A1cb", 1, 1)
            sh1cb = load_mod("sh1cb", 1, 0)
            hT = A("hT", [8, NTH * 128], BF16, nb=NTH, top=True)
            hcT = A("hcT", [8, 256], BF16, nb=2)
            xs1 = A("xs1", [2, D], F32, nb=2)
            tf1 = A("tf1", [2, D], F32, nb=2)
            tb1 = A("tb1", [2, D], BF16, nb=2)
            st1 = A("st1", [NTH + 2, 2], F32, nb=NTH + 2)
            for i in range(NTH + 2):
                s = i % 2
                if i < NTH:
                    src = xh[hbase + i * 128: hbase + (i + 1) * 128, :]
                    dst, dbuf, Ab, shb = hT.ap[:, :, i * 128:(i + 1) * 128], [hT.b[i]], A1b, sh1b
                else:
                    ci = i - NTH
                    src = ctxd[ci * 128:(ci + 1) * 128, :]
                    dst, dbuf, Ab, shb = hcT.ap[:, :, ci * 128:(ci + 1) * 128], [hcT.b[ci]], A1cb, sh1cb
                DMA('sp', xs1.ap[:, s], src, "L_%s_%d" % (xs1.name, s), w=[xs1.b[s]])
                norm_mod_T(xs1.ap[:, s], [xs1.b[s]], Ab, shb, st1, i, (tf1.ap[:, s], tf1.b[s]), (tb1.ap[:, s], tb1.b[s]),
                           6 + s, dst, dbuf)
            ar.release(A1b, sh1b, A1cb, sh1cb, xs1, tf1, tb1, st1)
            phase_end('n1', hf, {'hT': ((hT.ap, hT.b), [128, 8, NTH * 128]), 'hcT': ((hcT.ap, hcT.b), [128, 8, 256])})

            poolT = A("poolT", [8, HALF], BF16, nb=8, top=True)
            NU = HALF + 16
            uT = A("uT", [2, NU], F32)
            T1 = A("T1", [2, NU], F32)
            T2 = A("T2", [2, NU], F32)
            dT = A("dT", [2, HALF], BF16)
            wu = A("wu", [2, 8, 256], BF16, nb=2)
            wpool = A("wpool", [4, 2, 256], BF16)
            load(wpool, w_pool.rearrange("g (kc p) n -> p g kc n", p=128), cast=True)
            for g in range(4):
                s = g % 2
                DMA('pool', wu.ap[:, s], w_in[:, 1536 + g * 256: 1536 + (g + 1) * 256].rearrange("(k p) n -> p k n", p=128),
                    "L_%s_%d" % (wu.name, s), w=[wu.b[s]])
                bi = 0
                for mc in range(2):
                    for (a, b) in ((0, 512), (512, 1024), (1024, NU)):
                        pb = bi % 4
                        bi += 1
                        n = b - a
                        for k in range(8):
                            C('pe', mmf(bk(pb)[:, 0:n], wu.ap[:, s, k, mc * 128:(mc + 1) * 128],
                                        hT.ap[:, k, 120 + a:120 + b], k == 0, k == 7),
                              r=[wu.b[s]] + [hT.b[i] for i in tiles_of(120 + a, 120 + b)], w=[PB[pb]])
                        C('act', lambda e, pb=pb, n=n, mc=mc, a=a, b=b, g=g: e.activation(
                            out=uT.ap[:, mc, a:b], in_=bk(pb)[:, 0:n], func=AF.Identity,
                            bias=bcols.ap[:, g * 2 + mc: g * 2 + mc + 1]), r=[PB[pb]] + bcols.b, w=uT.b)
                C('dve', lambda e: e.tensor_scalar(out=uT.ap[:, :, 0:8], in0=uT.ap[:, :, 0:8],
                                                   scalar1=uv.ap[:, 2 * hf:2 * hf + 1], scalar2=None, op0=ALU.mult),
                  r=uT.b + uv.b, w=uT.b)
                C('dve', lambda e: e.tensor_scalar(out=uT.ap[:, :, NU - 8:NU], in0=uT.ap[:, :, NU - 8:NU],
                                                   scalar1=uv.ap[:, 2 * hf + 1:2 * hf + 2], scalar2=None, op0=ALU.mult),
                  r=uT.b + uv.b, w=uT.b)
                cur = uT
                tmps = [T1, T2]
                for n_ in range(1, g + 2):
                    d_ = 2 ** (n_ - 1)
                    lo = 2 ** n_ - 1
                    nxt = tmps[(n_ - 1) % 2]
                    C('dve', lambda e, cur=cur, nxt=nxt, d_=d_, lo=lo: e.tensor_tensor(
                        out=nxt.ap[:, :, lo:NU], in0=cur.ap[:, :, lo:NU], in1=cur.ap[:, :, lo - d_:NU - d_], op=ALU.add),
                      r=cur.b, w=nxt.b)
                    cur = nxt
                hw = 2 ** g
                wwin = 2 * hw
                off = 8 + hw - 1
                C('dve', lambda e, cur=cur, off=off, g=g: e.tensor_tensor(
                    out=cur.ap[:, :, off:off + 8], in0=cur.ap[:, :, off:off + 8],
                    in1=pcor.ap[:, hf, g, 0:8].unsqueeze(1).broadcast_to([128, 2, 8]), op=ALU.mult),
                  r=cur.b + pcor.b, w=cur.b)
                C('dve', lambda e, cur=cur, off=off, g=g: e.tensor_tensor(
                    out=cur.ap[:, :, off + HALF - 8:off + HALF], in0=cur.ap[:, :, off + HALF - 8:off + HALF],
                    in1=pcor.ap[:, hf, g, 8:16].unsqueeze(1).broadcast_to([128, 2, 8]), op=ALU.mult),
                  r=cur.b + pcor.b, w=cur.b)
                C('dve', lambda e, cur=cur, off=off, wwin=wwin: e.scalar_tensor_tensor(
                    out=dT.ap, in0=cur.ap[:, :, off:off + HALF], scalar=1.0 / wwin, in1=uT.ap[:, :, 8:8 + HALF],
                    op0=ALU.mult, op1=ALU.subtract), r=cur.b + uT.b, w=dT.b)
                for mc in range(2):
                    for tc in range(2):
                        pb = 4 + (mc * 2 + tc) % 2
                        for kc in range(2):
                            C('pe', mmf(bk(pb), wpool.ap[:, g, kc, mc * 128:(mc + 1) * 128],
                                        dT.ap[:, kc, tc * 512:(tc + 1) * 512], kc == 0, kc == 1),
                              r=wpool.b + dT.b, w=[PB[pb]])
                        C('act', lambda e, pb=pb, g=g, mc=mc, tc=tc: e.activation(
                            out=poolT.ap[:, 2 * g + mc, tc * 512:(tc + 1) * 512], in_=bk(pb), func=AF.Identity,
                            scale=pscol.ap[:, 2 * g + mc:2 * g + mc + 1]), r=[PB[pb]] + pscol.b, w=[poolT.b[2 * g + mc]])
            ar.release(uT, T1, T2, dT, wu, wpool)
            phase_end('pool', hf, {'poolT': ((poolT.ap, poolT.b), [128, 8, HALF])})

            masks = A("masks", [4, 512], BF16)
            load(masks, masksd.rearrange("p (a b) -> p a b", a=4), cast=True)
            rc = A("ropec", [18, 64], F32)
            load(rc, ropec.rearrange("p (a b) -> p a b", a=18))
            rs_ = A("ropes", [18, 64], F32)
            load(rs_, ropes.rearrange("p (a b) -> p a b", a=18))
            bqkv = A("bqkv", [1536], F32)
            load(bqkv, b_in[0:1, 0:1536].partition_broadcast(128))
            rowm = A("rowm", [2], F32)
            load(rowm, rowmaskd[:, :])
            m4 = A("m4", [512], F32)
            load(m4, m4d[:, :])
            esk = A("esk", [16], F32)
            load(esk, sink.partition_broadcast(128))
            C('act', lambda e, esk=esk: e.activation(out=esk.ap, in_=esk.ap, func=AF.Exp), r=esk.b, w=esk.b)
            eskB = A("eskB", [16, 128], F32)
            for h in range(16):
                C('dve', lambda e, h=h, esk=esk, eskB=eskB: e.tensor_scalar(
                    out=eskB.ap[:, h, :], in0=onesf.ap, scalar1=esk.ap[:, h:h + 1], scalar2=None, op0=ALU.mult),
                  r=esk.b + onesf.b, w=eskB.b)
            kT = A("kT", [4, NTH * 128], BF16, nb=NTH)
            vd = A("vd", [NTH, 4, 2, 64], BF16, nb=NTH)
            kcT = A("kcT", [4, 256], BF16, nb=2)
            vcd = A("vcd", [2, 4, 2, 64], BF16, nb=2)
            wkv = A("wkv", [8, 512], BF16)
            load(wkv, w_in[:, 1024:1536].rearrange("(k p) n -> p k n", p=128), cast=True)
            kvf = A("kvf", [2, 512], F32, nb=2)
            ra = A("ra", [2, 256], F32, nb=2)
            rb = A("rb", [2, 256], F32, nb=2)
            krd = A("krd", [2, 4, 2, 64], BF16, nb=2)
            for i in range(NTH + 2):
                s = i % 2
                isx = i < NTH
                ci = i - NTH
                gt = hf * 8 + i
                for k in range(8):
                    lh = hT.ap[:, k, i * 128:(i + 1) * 128] if isx else hcT.ap[:, k, ci * 128:(ci + 1) * 128]
                    C('pe', mmf(bk(s), lh, wkv.ap[:, k, :], k == 0, k == 7),
                      r=[hT.b[i] if isx else hcT.b[ci]] + wkv.b, w=[PB[s]])
                C('dve', lambda e, s=s: e.tensor_tensor(out=kvf.ap[:, s], in0=bk(s), in1=bqkv.ap[:, 1024:1536], op=ALU.add),
                  r=[PB[s]] + bqkv.b, w=[kvf.b[s]])
                vdst = vd.ap[:, i] if isx else vcd.ap[:, ci]
                vbuf = [vd.b[i]] if isx else [vcd.b[ci]]
                vsrc = kvf.ap[:, s, 256:512].rearrange("p (g d) -> p g d", g=4)
                C('act', lambda e, vdst=vdst, vsrc=vsrc: e.activation(out=vdst[:, :, 0, :], in_=vsrc, func=AF.Copy),
                  r=[kvf.b[s]], w=vbuf)
                C('act', lambda e, vdst=vdst, vsrc=vsrc: e.activation(out=vdst[:, :, 1, :], in_=vsrc, func=AF.Copy),
                  r=[kvf.b[s]], w=vbuf)
                ksrc = kvf.ap[:, s, 0:256]
                if isx:
                    k3 = ksrc.rearrange("p (g d) -> p g d", g=4)
                    k5 = ksrc.rearrange("p (g a h q) -> p g a h q", g=4, a=2, h=2)
                    ra3 = ra.ap[:, s].rearrange("p (g d) -> p g d", g=4)
                    rb5 = rb.ap[:, s].rearrange("p (g a h q) -> p g a h q", g=4, a=2, h=2)
                    sn4 = rs_.ap[:, gt].rearrange("p (a h q) -> p a h q", a=2, h=2)
                    C('dve', lambda e, k3=k3, ra3=ra3, gt=gt: e.tensor_tensor(
                        out=ra3, in0=k3, in1=rc.ap[:, gt].unsqueeze(1).broadcast_to([128, 4, 64]), op=ALU.mult),
                      r=[kvf.b[s]] + rc.b, w=[ra.b[s]])
                    for hh in range(2):
                        C('dve', lambda e, k5=k5, rb5=rb5, sn4=sn4, hh=hh: e.tensor_tensor(
                            out=rb5[:, :, :, hh, :], in0=k5[:, :, :, 1 - hh, :],
                            in1=sn4[:, :, hh, :].unsqueeze(1).broadcast_to([128, 4, 2, 16]), op=ALU.mult),
                          r=[kvf.b[s]] + rs_.b, w=[rb.b[s]])
                    for dd in range(2):
                        C('dve', lambda e, ra3=ra3, s=s, dd=dd: e.tensor_tensor(
                            out=krd.ap[:, s, :, dd, :], in0=ra3, in1=rb.ap[:, s].rearrange("p (g d) -> p g d", g=4),
                            op=ALU.add), r=[ra.b[s], rb.b[s]], w=[krd.b[s]])
                else:
                    k3 = ksrc.rearrange("p (g d) -> p g d", g=4)
                    for dd in range(2):
                        C('dve', lambda e, k3=k3, s=s, dd=dd: e.tensor_copy(out=krd.ap[:, s, :, dd, :], in_=k3),
                          r=[kvf.b[s]], w=[krd.b[s]])
                trb = 6 + s
                for g in range(4):
                    C('pe', lambda e, g=g, s=s, trb=trb: e.transpose(
                        out=bkb(trb)[:, g * 128:(g + 1) * 128],
                        in_=krd.ap[:, s, g].rearrange("p a d -> p (a d)"), identity=ident.ap),
                      r=[krd.b[s]] + ident.b, w=[PB[trb]])
                kdst = kT.ap[:, :, i * 128:(i + 1) * 128] if isx else kcT.ap[:, :, ci * 128:(ci + 1) * 128]
                C('act', lambda e, kdst=kdst, trb=trb: e.activation(
                    out=kdst, in_=bkb(trb)[:, 0:512].rearrange("p (g t) -> p g t", g=4), func=AF.Copy),
                  r=[PB[trb]], w=[kT.b[i]] if isx else [kcT.b[ci]])
            ar.release(wkv, kvf, ra, rb, krd, hcT)
            phase_end('kv', hf, {'kT': ((kT.ap, kT.b), [128, 4, NTH * 128]), 'vd': ((vd.ap, vd.b), [128, NTH, 4, 2, 64]), 'kcT': ((kcT.ap, kcT.b), [128, 4, 256])})

            attnT = A("attnT", [8, HALF], BF16, nb=NT, top=True)
            wq = A("wq", [8, D], BF16)
            load(wq, w_in[:, 0:1024].rearrange("(k p) n -> p k n", p=128), cast=True)
            qf = A("qf", [D], F32)
            qa = A("qa", [D], F32)
            qb = A("qb", [D], F32)
            qr = A("qr", [D], BF16)
            qT = A("qT", [2, 2, 8, 128], BF16, nb=2)
            prod = A("prod", [512], F32)
            PT = A("PT", [3, 5, 512], BF16, nb=3)
            rden = A("rden", [512], F32)
            sbi = [0]

            def att_prologue(t):
                i = t + 1
                gt = hf * 8 + i
                qs = t % 2
                for hh in range(2):
                    for k in range(8):
                        C('pe', mmf(bk(hh), hT.ap[:, k, i * 128:(i + 1) * 128], wq.ap[:, k, hh * 512:(hh + 1) * 512],
                                    k == 0, k == 7), r=[hT.b[i]] + wq.b, w=[PB[hh]])
                    C('dve', lambda e, hh=hh: e.tensor_tensor(out=qf.ap[:, hh * 512:(hh + 1) * 512], in0=bk(hh),
                                                              in1=bqkv.ap[:, hh * 512:(hh + 1) * 512], op=ALU.add),
                      r=[PB[hh]] + bqkv.b, w=qf.b)
                q3 = qf.ap.rearrange("p (g d) -> p g d", g=16)
                q5 = qf.ap.rearrange("p (g a h q) -> p g a h q", g=16, a=2, h=2)
                qa3 = qa.ap.rearrange("p (g d) -> p g d", g=16)
                qb5 = qb.ap.rearrange("p (g a h q) -> p g a h q", g=16, a=2, h=2)
                sn4 = rs_.ap[:, gt].rearrange("p (a h q) -> p a h q", a=2, h=2)
                C('dve', lambda e, q3=q3, qa3=qa3, gt=gt: e.tensor_tensor(
                    out=qa3, in0=q3, in1=rc.ap[:, gt].unsqueeze(1).broadcast_to([128, 16, 64]), op=ALU.mult),
                  r=qf.b + rc.b, w=qa.b)
                for hh in range(2):
                    C('dve', lambda e, q5=q5, qb5=qb5, sn4=sn4, hh=hh: e.tensor_tensor(
                        out=qb5[:, :, :, hh, :], in0=q5[:, :, :, 1 - hh, :],
                        in1=sn4[:, :, hh, :].unsqueeze(1).broadcast_to([128, 16, 2, 16]), op=ALU.mult),
                      r=qf.b + rs_.b, w=qb.b)
                C('dve', lambda e: e.tensor_tensor(out=qr.ap, in0=qa.ap, in1=qb.ap, op=ALU.add), r=qa.b + qb.b, w=qr.b)
                for c_ in range(8):
                    C('pe', lambda e, c_=c_: e.transpose(out=bkb(7)[:, c_ * 128:(c_ + 1) * 128],
                                                         in_=qr.ap[:, c_ * 128:(c_ + 1) * 128], identity=ident.ap),
                      r=qr.b + ident.b, w=[PB[7]])
                for par in range(2):
                    C('act', lambda e, qs=qs, par=par: e.activation(
                        out=qT.ap[:, qs, par], in_=bkb(7).rearrange("p (k t) -> p k t", k=8), func=AF.Identity,
                        scale=rowm.ap[:, par:par + 1]), r=[PB[7]] + rowm.b, w=[qT.b[qs]])

            def att_A(t, g, ps):
                qs = t % 2
                for kb in range(5):
                    sb_ = 2 + sbi[0] % 2
                    sbi[0] += 1
                    for h in range(4):
                        c_ = 2 * g + h // 2
                        if kb < 3:
                            kl = kT.ap[:, g, (t + kb) * 128:(t + kb + 1) * 128]
                            kbuf = [kT.b[t + kb]]
                        else:
                            kl = kcT.ap[:, g, (kb - 3) * 128:(kb - 2) * 128]
                            kbuf = [kcT.b[kb - 3]]
                        C('pe', mmf(bk(sb_)[:, h * 128:(h + 1) * 128], kl, qT.ap[:, qs, h % 2, c_, :]),
                          r=kbuf + [qT.b[qs]], w=[PB[sb_]])
                    C('act', lambda e, sb_=sb_, ps=ps, kb=kb: e.activation(
                        out=PT.ap[:, ps, kb, :], in_=bk(sb_), func=AF.Exp, scale=0.125), r=[PB[sb_]], w=[PT.b[ps]])
                    if kb in (0, 2):
                        if kb == 0:
                            mi = 2 if (hf == 0 and t == 0) else 0
                        else:
                            mi = 3 if (hf == 1 and t == NT - 1) else 1
                        C('dve', lambda e, ps=ps, kb=kb, mi=mi: e.tensor_tensor(
                            out=PT.ap[:, ps, kb, :], in0=PT.ap[:, ps, kb, :], in1=masks.ap[:, mi, :], op=ALU.mult),
                          r=[PT.b[ps]] + masks.b, w=[PT.b[ps]])

            def att_B(t, g, ps, ub):
                for kb in range(5):
                    if kb < 3:
                        vl = vd.ap[:, t + kb, g].rearrange("p a d -> p (a d)")
                        vbuf = [vd.b[t + kb]]
                    else:
                        vl = vcd.ap[:, kb - 3, g].rearrange("p a d -> p (a d)")
                        vbuf = [vcd.b[kb - 3]]
                    C('pe', mmf(bk(ub), vl, PT.ap[:, ps, kb, :], kb == 0, kb == 4), r=vbuf + [PT.b[ps]], w=[PB[ub]])
                for kb in range(5):
                    C('pe', mmf(bk(6), onesb.ap, PT.ap[:, ps, kb, :], kb == 0, kb == 4),
                      r=onesb.b + [PT.b[ps]], w=[PB[6]])
                C('dve', lambda e, g=g: e.tensor_tensor(
                    out=rden.ap, in0=bk(6), in1=eskB.ap[:, 4 * g:4 * g + 4, :].rearrange("p h q -> p (h q)"),
                    op=ALU.add), r=[PB[6]] + eskB.b, w=rden.b)
                C('dve', lambda e: e.reciprocal(out=rden.ap, in_=rden.ap), r=rden.b, w=rden.b)
                C('dve', lambda e: e.tensor_tensor(out=rden.ap, in0=rden.ap, in1=m4.ap, op=ALU.mult),
                  r=rden.b + m4.b, w=rden.b)
                C('dve', lambda e, ub=ub: e.tensor_tensor(out=prod.ap, in0=bk(ub), in1=rden.ap, op=ALU.mult),
                  r=[PB[ub]] + rden.b, w=prod.b)
                p4 = prod.ap.rearrange("p (c par q) -> p c par q", c=2, par=2)
                C('dve', lambda e, p4=p4, g=g, t=t: e.tensor_tensor(
                    out=attnT.ap[:, 2 * g:2 * g + 2, t * 128:(t + 1) * 128], in0=p4[:, :, 0, :], in1=p4[:, :, 1, :],
                    op=ALU.add), r=prod.b, w=[attnT.b[t]])

            groups = [(t, g) for t in range(NT) for g in range(4)]
            for idx in range(len(groups) + 1):
                if idx < len(groups):
                    t, g = groups[idx]
                    if g == 0:
                        att_prologue(t)
                    att_A(t, g, idx % 3)
                if idx >= 1:
                    t_, g_ = groups[idx - 1]
                    att_B(t_, g_, (idx - 1) % 3, 4 + (idx - 1) % 2)
            phase_end('att', hf, {'attnT': ((attnT.ap, attnT.b), [128, 8, HALF]), 'poolT': ((poolT.ap, poolT.b), [128, 8, HALF]), 'kT': ((kT.ap, kT.b), [128, 4, NTH * 128]), 'vd': ((vd.ap, vd.b), [128, NTH, 4, 2, 64]), 'kcT': ((kcT.ap, kcT.b), [128, 4, 256])})
            ar.release(wq, qf, qa, qb, qr, qT, PT, rden, kT, vd, kcT, vcd, masks, rc, rs_, bqkv, esk, eskB, rowm, m4, prod)

            mT = A("mT", [8, HALF], BF16, nb=NT, top=True)
            wga = A("wga", [8, D], BF16)
            wgp = A("wgp", [8, D], BF16)
            wba = A("wba", [8, D], BF16)
            wbp = A("wbp", [8, D], BF16)
            load(wga, w_in[:, 2560:3584].rearrange("(k p) n -> p k n", p=128), cast=True)
            load(wba, w_abr.rearrange("(k p) n -> p k n", p=128), cast=True)
            load(wgp, w_in[:, 3584:4608].rearrange("(k p) n -> p k n", p=128), cast=True)
            load(wbp, w_pbr.rearrange("(k p) n -> p k n", p=128), cast=True)
            sga = A("sga", [2, 512], F32, nb=2)
            sgp = A("sgp", [2, 512], F32, nb=2)
            t1 = A("t1", [2, 512], F32, nb=2)
            t2 = A("t2", [2, 512], F32, nb=2)
            it = 0
            for tc in range(2):
                hcols = slice(128 + tc * 512, 128 + (tc + 1) * 512)
                hbufs = [hT.b[i] for i in tiles_of(128 + tc * 512, 128 + (tc + 1) * 512)]
                tcs = slice(tc * 512, (tc + 1) * 512)
                tbufs = list(range(tc * 4, tc * 4 + 4))
                for m in range(8):
                    s = it % 2
                    it += 1
                    b0 = 4 * s
                    ms = slice(m * 128, (m + 1) * 128)
                    for k in range(8):
                        C('pe', mmf(bk(b0), wga.ap[:, k, ms], hT.ap[:, k, hcols], k == 0, k == 7),
                          r=wga.b + hbufs, w=[PB[b0]])
                    C('act', lambda e, b0=b0, s=s, m=m: e.activation(out=sga.ap[:, s], in_=bk(b0), func=AF.Sigmoid,
                                                                     bias=bcols.ap[:, 8 + m:9 + m]),
                      r=[PB[b0]] + bcols.b, w=[sga.b[s]])
                    for k in range(8):
                        C('pe', mmf(bk(b0 + 1), wgp.ap[:, k, ms], hT.ap[:, k, hcols], k == 0, k == 7),
                          r=wgp.b + hbufs, w=[PB[b0 + 1]])
                    C('act', lambda e, b0=b0, s=s, m=m: e.activation(out=sgp.ap[:, s], in_=bk(b0 + 1), func=AF.Sigmoid,
                                                                     bias=bcols.ap[:, 16 + m:17 + m]),
                      r=[PB[b0 + 1]] + bcols.b, w=[sgp.b[s]])
                    for k in range(8):
                        C('pe', mmf(bk(b0 + 2), wba.ap[:, k, ms], attnT.ap[:, k, tcs], k == 0, k == 7),
                          r=wba.b + [attnT.b[j] for j in tbufs], w=[PB[b0 + 2]])
                    for k in range(8):
                        C('pe', mmf(bk(b0 + 3), wbp.ap[:, k, ms], poolT.ap[:, k, tcs], k == 0, k == 7),
                          r=wbp.b + poolT.b, w=[PB[b0 + 3]])
                    C('dve', lambda e, b0=b0, s=s: e.tensor_tensor(out=t1.ap[:, s], in0=bk(b0 + 2), in1=sga.ap[:, s],
                                                                   op=ALU.mult), r=[PB[b0 + 2], sga.b[s]], w=[t1.b[s]])
                    C('dve', lambda e, b0=b0, s=s: e.tensor_tensor(out=t2.ap[:, s], in0=bk(b0 + 3), in1=sgp.ap[:, s],
                                                                   op=ALU.mult), r=[PB[b0 + 3], sgp.b[s]], w=[t2.b[s]])
                    C('dve', lambda e, s=s, m=m, tcs=tcs: e.tensor_tensor(out=mT.ap[:, m, tcs], in0=t1.ap[:, s],
                                                                         in1=t2.ap[:, s], op=ALU.add),
                      r=[t1.b[s], t2.b[s]], w=[mT.b[j] for j in tbufs])
            phase_end('br', hf, {'mT': ((mT.ap, mT.b), [128, 8, HALF])})
            ar.release(wga, wgp, wba, wbp, sga, sgp, t1, t2, attnT, poolT, hT)

            G1b = load_mod("G1b", 0, 2)
            A2b = load_mod("A2b", 0, 4)
            sh2b = load_mod("sh2b", 0, 3)
            wo = A("wo", [8, D], BF16)
            load(wo, w_outd.rearrange("(k p) n -> p k n", p=128), cast=True)
            h2T = A("h2T", [8, HALF], BF16, nb=NT, top=True)
            Gt = A("G", [NT, NE], F32, nb=NT, top=True)
            Gs = A("Gs", [NT, NE], F32, nb=NT, top=True)
            x1o = A("x1o", [2, D], F32, nb=2)
            GT = A("GT", [NT, 128], BF16, nb=NT, top=True)
            xs = A("xs", [2, D], F32, nb=2)
            tf = A("tf", [2, D], F32, nb=2)
            tb = A("tb", [2, D], BF16, nb=2)
            st2 = A("st2", [NT, 2], F32, nb=NT)
            lgA = A("lgA", [NT, NE], F32, nb=NT)
            t8A = A("t8A", [NT, 8], F32, nb=NT)
            rwk = A("rwk", [3, NT, NE], F32)
            rsm = A("rsm", [2, NT], F32)
            gbf = A("gbf", [NT, 128], BF16)
            C('pool', lambda e, gbf=gbf: e.memset(gbf.ap, 0.0), w=gbf.b)
            for t in range(NT):
                s = t % 2
                row0 = 128 + hbase + t * 128
                orow = hbase + t * 128
                DMA('sp', xs.ap[:, s], xh[row0:row0 + 128, :], "L_%s_%d" % (xs.name, s), w=[xs.b[s]])
                for hh in range(2):
                    pb = 2 * s + hh
                    hs = slice(hh * 512, (hh + 1) * 512)
                    for k in range(8):
                        C('pe', mmf(bk(pb), mT.ap[:, k, t * 128:(t + 1) * 128], wo.ap[:, k, hs], k == 0, k == 7),
                          r=[mT.b[t]] + wo.b, w=[PB[pb]])
                    C('dve', lambda e, pb=pb, s=s, hs=hs: e.tensor_tensor(out=tf.ap[:, s, hs], in0=bk(pb), in1=G1b.ap[:, hs],
                                                                         op=ALU.mult), r=[PB[pb]] + G1b.b, w=[tf.b[s]])
                    C('dve', lambda e, s=s, hs=hs: e.tensor_tensor(out=x1o.ap[:, s, hs], in0=tf.ap[:, s, hs],
                                                                   in1=xs.ap[:, s, hs], op=ALU.add),
                      r=[tf.b[s], xs.b[s]], w=[x1o.b[s]])
                DMA('sp', outd[orow:orow + 128, :], x1o.ap[:, s], "S_x1", r=[x1o.b[s]], w=[x1B[hf * 8 + t]])
                norm_mod_T(x1o.ap[:, s], [x1o.b[s]], A2b, sh2b, st2, t, (tf.ap[:, s], tf.b[s]), (tb.ap[:, s], tb.b[s]),
                           6 + s, h2T.ap[:, :, t * 128:(t + 1) * 128], [h2T.b[t]])
                for k in range(8):
                    C('pe', mmf(bk(4)[:, 0:NE], h2T.ap[:, k, t * 128:(t + 1) * 128], wr.ap[:, k, :], k == 0, k == 7),
                      r=[h2T.b[t]] + wr.b, w=[PB[4]])
                C('dve', lambda e, t=t: e.tensor_tensor(out=lgA.ap[:, t, :], in0=bk(4)[:, 0:NE], in1=brt.ap, op=ALU.add),
                  r=[PB[4]] + brt.b, w=[lgA.b[t]])
                C('dve', lambda e, t=t: e.max(out=t8A.ap[:, t, :], in_=lgA.ap[:, t, :]), r=[lgA.b[t]], w=[t8A.b[t]])
            mkA, exA, exmA = (rwk.ap[:, j] for j in range(3))
            C('dve', lambda e: e.tensor_tensor(out=mkA, in0=lgA.ap, in1=t8A.ap[:, :, 3:4].broadcast_to([128, NT, NE]),
                                               op=ALU.is_ge), r=lgA.b + t8A.b, w=rwk.b)
            C('dve', lambda e: e.tensor_tensor(out=exA, in0=lgA.ap, in1=t8A.ap[:, :, 0:1].broadcast_to([128, NT, NE]),
                                               op=ALU.subtract), r=lgA.b + t8A.b, w=rwk.b)
            C('act', lambda e: e.activation(out=exA, in_=exA, func=AF.Exp), r=rwk.b, w=rwk.b)
            C('dve', lambda e: e.tensor_tensor(out=exmA, in0=exA, in1=mkA, op=ALU.mult), r=rwk.b, w=rwk.b)
            C('dve', lambda e: e.reduce_sum(out=rsm.ap[:, 0, :], in_=exmA, axis=AX.X), r=rwk.b, w=rsm.b)
            C('dve', lambda e: e.reciprocal(out=rsm.ap[:, 1, :], in_=rsm.ap[:, 0, :]), r=rsm.b, w=rsm.b)
            C('dve', lambda e: e.tensor_tensor(out=Gt.ap, in0=exmA, in1=rsm.ap[:, 1, :].unsqueeze(2).broadcast_to([128, NT, NE]),
                                               op=ALU.mult), r=rwk.b + rsm.b, w=Gt.b)
            C('dve', lambda e: e.tensor_scalar(out=Gs.ap, in0=Gt.ap, scalar1=1.0 / 1.702, scalar2=None, op0=ALU.mult),
              r=Gt.b, w=Gs.b)
            C('dve', lambda e: e.tensor_copy(out=gbf.ap[:, :, 0:NE], in_=Gt.ap), r=Gt.b, w=gbf.b)
            for t in range(NT):
                C('pe', lambda e, t=t: e.transpose(out=bkb(5)[:, t * 128:(t + 1) * 128], in_=gbf.ap[:, t, :], identity=ident.ap),
                  r=gbf.b + ident.b, w=[PB[5]])
            C('act', lambda e: e.activation(out=GT.ap, in_=bkb(5).rearrange("p (t q) -> p t q", t=NT), func=AF.Copy),
              r=[PB[5]], w=GT.b)
            phase_end('out', hf, {'h2T': ((h2T.ap, h2T.b), [128, 8, HALF]), 'G': ((Gt.ap, Gt.b), [128, NT, NE])})
            ar.release(G1b, A2b, sh2b, wo, xs, x1o, tf, tb, st2, lgA, t8A, rwk, rsm, gbf, mT)

            acc = A("acc", [NT, D], F32, nb=NT, top=True)
            wgu = A("wgu", [2, 4, 2, 2 * D], BF16, nb=8)
            wdn = A("wdn", [1, 2, 4, D], BF16, nb=2)
            for t in range(NT):
                for hh in range(2):
                    pb = 4 + (t * 2 + hh) % 4
                    hs = slice(hh * 512, (hh + 1) * 512)
                    C('pe', mmf(bk(pb), GT.ap[:, t, :], bdn.ap[:, hs]), r=[GT.b[t]] + bdn.b, w=[PB[pb]])
                    C('act', lambda e, pb=pb, t=t, hs=hs: e.activation(out=acc.ap[:, t, hs], in_=bk(pb), func=AF.Copy),
                      r=[PB[pb]], w=[acc.b[t]])
            actT = A("actT", [2, 8, 512], BF16, nb=2)
            gc = A("gc", [2, 512], F32, nb=2)
            sg = A("sg", [2, 512], F32, nb=2)
            lc = A("lc", [2, 512], F32, nb=2)
            yi = 0
            pi = 0
            for e_ in range(NEXP):
                es = e_ % 2
                gsrc = w_gu[e_].rearrange("(k p) n -> p k n", p=128)
                dsrc = w_down[e_].rearrange("(k p) n -> p k n", p=128)
                for j in range(4):
                    DMA('pool', wgu.ap[:, es, j], gsrc[:, 2 * j:2 * j + 2, :], "L_%s_%d_%d" % (wgu.name, es, j), w=[wgu.b[es * 4 + j]])
                for j in range(2):
                    DMA('pool', wdn.ap[:, 0, j], dsrc[:, 4 * j:4 * j + 4, :], "L_%s_%d_%d" % (wdn.name, es, j), w=[wdn.b[j]])
                for tc in range(2):
                    as_ = (e_ * 2 + tc) % 2
                    tcs = slice(tc * 512, (tc + 1) * 512)
                    hb_ = [h2T.b[j] for j in range(tc * 4, tc * 4 + 4)]
                    for mp in range(8):
                        s = pi % 2
                        pi += 1
                        bg, bl = 2 * s, 2 * s + 1
                        for k in range(8):
                            C('pe', mmf(bk(bg), wgu.ap[:, es, k // 2, k % 2, mp * 128:(mp + 1) * 128], h2T.ap[:, k, tcs],
                                        k == 0, k == 7), r=[wgu.b[es * 4 + k // 2]] + hb_, w=[PB[bg]])
                        for k in range(8):
                            C('pe', mmf(bk(bl), wgu.ap[:, es, k // 2, k % 2, D + mp * 128:D + (mp + 1) * 128],
                                        h2T.ap[:, k, tcs], k == 0, k == 7), r=[wgu.b[es * 4 + k // 2]] + hb_, w=[PB[bl]])
                        C('dve', lambda e, bg=bg, s=s, e_=e_, mp=mp: e.tensor_scalar(
                            out=gc.ap[:, s], in0=bk(bg), scalar1=bguc.ap[:, e_, mp:mp + 1], scalar2=7.0,
                            op0=ALU.add, op1=ALU.min), r=[PB[bg]] + bguc.b, w=[gc.b[s]])
                        C('act', lambda e, s=s: e.activation(out=sg.ap[:, s], in_=gc.ap[:, s], func=AF.Silu, scale=1.702),
                          r=[gc.b[s]], w=[sg.b[s]])
                        C('dve', lambda e, bl=bl, s=s, e_=e_, mp=mp: e.tensor_scalar(
                            out=lc.ap[:, s], in0=bk(bl), scalar1=bguc.ap[:, e_, 8 + mp:9 + mp], scalar2=8.0,
                            op0=ALU.add, op1=ALU.min), r=[PB[bl]] + bguc.b, w=[lc.b[s]])
                        C('dve', lambda e, s=s, as_=as_, mp=mp: e.scalar_tensor_tensor(
                            out=actT.ap[:, as_, mp, :], in0=lc.ap[:, s], scalar=-6.0, in1=sg.ap[:, s],
                            op0=ALU.max, op1=ALU.mult), r=[lc.b[s], sg.b[s]], w=[actT.b[as_]])
                    for tt in range(4):
                        t = tc * 4 + tt
                        for hh in range(2):
                            pb = 4 + yi % 4
                            yi += 1
                            hs = slice(hh * 512, (hh + 1) * 512)
                            for k in range(8):
                                C('pe', mmf(bk(pb), actT.ap[:, as_, k, tt * 128:(tt + 1) * 128],
                                            wdn.ap[:, 0, k // 4, k % 4, hs], k == 0, k == 7),
                                  r=[actT.b[as_], wdn.b[k // 4]], w=[PB[pb]])
                            C('dve', lambda e, pb=pb, t=t, hs=hs, e_=e_: e.scalar_tensor_tensor(
                                out=acc.ap[:, t, hs], in0=bk(pb), scalar=Gs.ap[:, t, e_:e_ + 1], in1=acc.ap[:, t, hs],
                                op0=ALU.mult, op1=ALU.add), r=[PB[pb], Gs.b[t], acc.b[t]], w=[acc.b[t]])
            phase_end('moe', hf, {'acc': ((acc.ap, acc.b), [128, NT, D])})
            ar.release(wgu, wdn, actT, gc, sg, lc, h2T, Gt, Gs, GT)

            G2b = load_mod("G2b", 0, 5)
            fgb = A("fgb", [D], F32)
            load(fgb, fg.partition_broadcast(128))
            xr = A("xr", [2, D], F32, nb=2)
            xo = A("xo", [2, D], F32, nb=2)
            tfo = A("tfo", [2, D], F32, nb=2)
            st3 = A("st3", [NT, 2], F32, nb=NT)
            for t in range(NT):
                s = t % 2
                orow = hbase + t * 128
                DMA('sp', xr.ap[:, s], outd[orow:orow + 128, :], "L_xr", r=[x1B[hf * 8 + t]], w=[xr.b[s]])
                C('dve', lambda e, s=s, t=t, acc=acc, G2b=G2b, tfo=tfo: e.tensor_tensor(
                    out=tfo.ap[:, s], in0=acc.ap[:, t, :], in1=G2b.ap, op=ALU.mult), r=[acc.b[t]] + G2b.b, w=[tfo.b[s]])
                C('dve', lambda e, s=s, xo=xo, tfo=tfo, xr=xr: e.tensor_tensor(
                    out=xo.ap[:, s], in0=tfo.ap[:, s], in1=xr.ap[:, s], op=ALU.add), r=[tfo.b[s], xr.b[s]], w=[xo.b[s]])
                norm_mod_T(xo.ap[:, s], [xo.b[s]], fgb, None, st3, t, (tfo.ap[:, s], tfo.b[s]), None, None, None, None)
                final_ops.append(DMA('sp', outd[orow:orow + 128, :], tfo.ap[:, s], "S_out", r=[tfo.b[s]], w=[x1B[hf * 8 + t]]))
            phase_end('fin', hf, {'tfin': ((tfo.ap, tfo.b), [128, 2, D]), 'accf': ((acc.ap, acc.b), [128, NT, D])})
            ar.release(G2b, fgb, xr, xo, tfo, st3, acc)

    except _Stop:
        pass
    P.emit(nc, final_ops=final_ops)
    return nc


def build_program_safe(stop=None, dumps=None):
    return build_program(stop, dumps)


def _host_consts(j):
    L = 8192
    ident = np.eye(128, dtype=np.float32)
    jj = np.arange(128)[:, None]
    ii = np.arange(128)[None, :]
    mP = (jj >= ii).astype(np.float32)
    mN = (jj <= ii).astype(np.float32)
    mPF = mP if j != 0 else np.zeros_like(mP)
    mNL = mN if j != 3 else np.zeros_like(mN)
    masks = np.concatenate([np.tile(m, (1, 4)) for m in (mP, mN, mPF, mNL)], axis=1).astype(np.float32)
    pos = j * 2048 - 128 + np.arange(2304)
    posc = np.clip(pos, 0, L - 1)
    row = (posc // 64).astype(np.float32)
    col = (posc % 64).astype(np.float32)
    inv_freq = (np.float32(10000.0) ** (-np.arange(16, dtype=np.float32) / np.float32(16))).astype(np.float32)
    ang = np.stack([row[:, None] * inv_freq, col[:, None] * inv_freq], axis=1).astype(np.float32)
    cs, sn = np.cos(ang).astype(np.float32), np.sin(ang).astype(np.float32)
    cos64 = np.stack([cs, cs], axis=2).reshape(2304, 64)
    sin64 = np.stack([-sn, sn], axis=2).reshape(2304, 64)
    ropec = cos64.reshape(18, 128, 64).transpose(1, 0, 2).reshape(128, 18 * 64)
    ropes = sin64.reshape(18, 128, 64).transpose(1, 0, 2).reshape(128, 18 * 64)
    pcorr = np.ones((2, 4, 16), np.float32)
    uval = np.ones((2, 2), np.float32)
    for hf in range(2):
        p0 = j * 2048 + hf * 1024
        for g in range(4):
            h = 2 ** g
            w = 2 * h
            for side, ts in ((0, range(0, 8)), (1, range(1016, 1024))):
                for q, t in enumerate(ts):
                    p = p0 + t
                    cnt = min(p + h, L) - max(p - h, 0)
                    pcorr[hf, g, side * 8 + q] = w / cnt
        if p0 == 0:
            uval[hf, 0] = 0.0
        if p0 + 1024 == L:
            uval[hf, 1] = 0.0
    rowmask = np.zeros((128, 2), np.float32)
    rowmask[:64, 0] = 1.0
    rowmask[64:, 1] = 1.0
    m4 = np.zeros((128, 4, 128), np.float32)
    for h in range(4):
        m4[:, h, :] = rowmask[:, h % 2][:, None]
    return dict(rowmask=rowmask, m4=m4.reshape(128, 512), ident=ident, masks=masks, ropec=np.ascontiguousarray(ropec), ropes=np.ascontiguousarray(ropes),
                pcorr=pcorr.reshape(1, -1), uval=uval.reshape(1, -1))


def make_in_maps(x, c, ctx, c_ctx, w_ada, b_ada, norm1_g, norm2_g, w_in, b_in, attn_sink, w_pool, pool_scale,
                 w_attn_br, w_pool_br, w_out, w_router, b_router, w_gu, b_gu, w_down, b_down, final_g):
    f = lambda a: np.ascontiguousarray(np.asarray(a, dtype=np.float32))
    x, c, ctx, c_ctx = f(x), f(c), f(ctx), f(c_ctx)
    shared = dict(
        w_ada=f(w_ada[0]), b_ada=f(b_ada[0]).reshape(1, -1), n1g=f(norm1_g[0]).reshape(1, -1),
        n2g=f(norm2_g[0]).reshape(1, -1), fg=f(final_g).reshape(1, -1), w_in=f(w_in[0]),
        b_in=f(b_in[0]).reshape(1, -1), b_in_cols=f(np.asarray(b_in[0])[1536:].reshape(24, 128).T),
        sink=f(attn_sink[0]).reshape(1, -1), w_pool=f(w_pool[0]),
        pscale_cols=f(np.asarray(pool_scale[0]).reshape(8, 128).T), w_abr=f(w_attn_br[0]), w_pbr=f(w_pool_br[0]),
        w_out=f(w_out[0]), w_router=f(w_router[0]), b_router=f(b_router[0]).reshape(1, -1), w_gu=f(w_gu[0]),
        b_gu_cols=f(np.asarray(b_gu[0]).reshape(NE, 16, 128).transpose(2, 0, 1).reshape(128, NE * 16)),
        w_down=f(w_down[0]), b_down=f(b_down[0]))
    maps = []
    for r in range(8):
        b, j = r // 4, r % 4
        xhalo = np.zeros((2304, D), np.float32)
        lo, hi = j * 2048 - 128, (j + 1) * 2048 + 128
        slo, shi = max(lo, 0), min(hi, 8192)
        xhalo[slo - lo: shi - lo] = x[b, slo:shi]
        ccols = np.stack([c[b].reshape(8, 128).T, c_ctx.reshape(8, 128).T], axis=2).reshape(128, 16)
        m = dict(shared)
        m.update(xh=xhalo, ctx=f(ctx[b]), ccols=f(ccols))
        m.update(_host_consts(j))
        maps.append(m)
    return maps


_NC_CACHE = {}


def kernel(**inputs):
    if "nc" not in _NC_CACHE:
        _NC_CACHE["nc"] = build_program()
    nc = _NC_CACHE["nc"]
    maps = make_in_maps(**inputs)
    res = run_bass_kernel_spmd(nc, maps, core_ids=list(range(8)))
    out = np.stack([r["out"] for r in res.results], axis=0)
    return out.reshape(2, 4, 2048, D).reshape(2, 8192, D).astype(np.float32)
```

```python
import contextlib
import numpy as np
import concourse.bass as bass
import concourse.mybir as mybir
from concourse.bass_utils import run_bass_kernel_spmd

F32 = mybir.dt.float32
BF16 = mybir.dt.bfloat16
AF = mybir.ActivationFunctionType
ALU = mybir.AluOpType
AX = mybir.AxisListType

D = 1024
NE = 32
HALF = 1024
NT = 8
NTH = 10
DEBUG = False
import os
NEXP = int(os.environ.get('NEXP', '32'))


class Buf:
    __slots__ = ("name", "lastw", "readers")

    def __init__(self, name):
        self.name = name
        self.lastw = None
        self.readers = []


class Op:
    __slots__ = ("eng", "fn", "deps", "kind", "dsem", "ndep", "tok", "inc")

    def __init__(self, eng, fn, kind, dsem=None):
        self.eng = eng
        self.fn = fn
        self.kind = kind
        self.dsem = dsem
        self.deps = []
        self.ndep = 0
        self.tok = None
        self.inc = False


class Prog:
    ENGS = ("pe", "act", "dve", "pool", "sp")

    def __init__(self):
        self.ops = {e: [] for e in self.ENGS}
        self.dma_sems = []

    def _add(self, op, reads, writes):
        deps = []
        for b in reads:
            w = b.lastw
            if w is not None:
                deps.append(w)
        for b in writes:
            w = b.lastw
            if w is not None:
                if w.kind == 'c' and op.kind == 'c' and w.eng == op.eng:
                    pass
                else:
                    deps.append(w)
            for r in b.readers:
                if r.kind == 'c' and op.kind == 'c' and r.eng == op.eng:
                    continue
                deps.append(r)
        seen = set()
        for d in deps:
            if d is op or id(d) in seen:
                continue
            seen.add(id(d))
            if d.kind == 'c' and op.kind == 'c' and d.eng == 'pe' and op.eng == 'pe':
                continue
            op.deps.append(d)
            d.ndep += 1
        for b in writes:
            b.lastw = op
            b.readers = []
        for b in reads:
            if b.lastw is not op:
                b.readers.append(op)
        self.ops[op.eng].append(op)
        return op

    def c(self, eng, fn, reads=(), writes=()):
        return self._add(Op(eng, fn, 'c'), reads, writes)

    def dma(self, eng, fn, dsem, reads=(), writes=()):
        if dsem not in self.dma_sems:
            self.dma_sems.append(dsem)
        return self._add(Op(eng, fn, 'd', dsem), reads, writes)

    def emit(self, nc, final_ops=()):
        CH = 8000
        NPOOL = 16
        with contextlib.ExitStack() as st:
            esems = {}
            sems = {}
            for e in self.ENGS:
                n = 0
                nd = 0
                deferred = []
                for op in self.ops[e]:
                    if op.kind == 'd':
                        key = ("dma", e, nd % NPOOL)
                        nd += 1
                        if key not in sems:
                            sems[key] = [st.enter_context(nc.semaphore("d_%s%d" % (e, key[2]))), 0]
                        op.dsem = (key, sems[key][1])
                        sems[key][1] += 16
                        op.tok = (key, sems[key][1])
                    elif e == 'pe' and getattr(op.fn, 'stop', None) is False:
                        if op.ndep > 0:
                            deferred.append(op)
                    elif op.ndep > 0 or deferred:
                        n += 1
                        key = (e, (n - 1) // CH)
                        if key not in sems:
                            sems[key] = [st.enter_context(nc.semaphore("e_%s%d" % key)), 0]
                        op.tok = (key, (n - 1) % CH + 1)
                        op.inc = True
                        for d_ in deferred:
                            d_.tok = op.tok
                        deferred = []
                assert not deferred
            block = st.enter_context(nc.Block())

            def run(eng_name, eng):
                waited = {}
                for op in self.ops[eng_name]:
                    need = {}
                    for d in op.deps:
                        k, v = d.tok
                        if waited.get(k, 0) < v and need.get(k, 0) < v:
                            need[k] = v
                    if op.kind == 'd':
                        k, v = op.dsem
                        if v > 0 and waited.get(k, 0) < v and need.get(k, 0) < v:
                            need[k] = v
                    for k, v in need.items():
                        waited[k] = v
                        eng.wait_ge(sems[k][0], v)
                    ins = op.fn(eng)
                    if op.kind == 'd':
                        ins.then_inc(sems[op.tok[0]][0], 16)
                    elif op.inc:
                        ins.then_inc(sems[op.tok[0]][0], 1)
                if eng_name == 'sp':
                    for d in final_ops:
                        k, v = d.tok
                        eng.wait_ge(sems[k][0], v)

            block.tensor(lambda eng: run('pe', eng))
            block.scalar(lambda eng: run('act', eng))
            block.vector(lambda eng: run('dve', eng))
            block.gpsimd(lambda eng: run('pool', eng))
            block.sync(lambda eng: run('sp', eng))
            self.nsems = len(sems)


class T:
    __slots__ = ("name", "ap", "b", "start", "end", "flat", "uid")


class Arena:
    def __init__(self, nc, nbytes):
        self.nbytes = nbytes
        self.t = nc.sbuf_tensor("arena", [128, nbytes // 2], BF16).__enter__()
        self.free = [(0, nbytes)]
        self.grave = []
        self.live = {}
        self.peak = 0
        self.uid = 0

    def alloc(self, name, shape, dt, nb=1, parts=128, top=False):
        n = 1
        for s in shape:
            n *= s
        nbytes = n * (4 if dt == F32 else 2)
        nbytes = (nbytes + 63) // 64 * 64
        order = range(len(self.free) - 1, -1, -1) if top else range(len(self.free))
        for i in order:
            s, e = self.free[i]
            if e - s >= nbytes:
                if top:
                    self.free[i] = (s, e - nbytes)
                    s = e - nbytes
                else:
                    self.free[i] = (s + nbytes, e)
                if self.free[i][0] == self.free[i][1]:
                    del self.free[i]
                break
        else:
            raise RuntimeError("arena OOM for %s (%d B); live=%s" % (name, nbytes, sorted(
                (v_.end - v_.start, k) for k, v_ in self.live.items())))
        t = T()
        self.uid += 1
        t.uid = self.uid
        t.name = "%s_%d" % (name, self.uid)
        t.start, t.end = s, s + nbytes
        self.peak = max(self.peak, t.end)
        v = self.t[0:parts, s // 2:(s + nbytes) // 2]
        if dt != BF16:
            v = v.bitcast(dt)
        v = v[:, 0:n]
        t.flat = v
        if len(shape) == 2:
            v = v.rearrange("p (a b) -> p a b", a=shape[0])
        elif len(shape) == 3:
            v = v.rearrange("p (a b c) -> p a b c", a=shape[0], b=shape[1])
        elif len(shape) == 4:
            v = v.rearrange("p (a b c d) -> p a b c d", a=shape[0], b=shape[1], c=shape[2])
        t.ap = v
        t.b = [Buf("%s.%d" % (name, i)) for i in range(nb)]
        inh = []
        for (gs, ge, bufs) in self.grave:
            if gs < t.end and ge > t.start:
                for gb in bufs:
                    if gb.lastw is not None:
                        inh.append(gb.lastw)
                    inh.extend(gb.readers)
        if inh:
            seen = set()
            u = []
            for o in inh:
                if id(o) not in seen:
                    seen.add(id(o))
                    u.append(o)
            for b in t.b:
                b.readers = list(u)
        self.live[t.name] = t
        return t

    def release(self, *ts):
        for t in ts:
            del self.live[t.name]
            self.grave.append((t.start, t.end, t.b))
            self.free.append((t.start, t.end))
        self.free.sort()
        m = []
        for s, e in self.free:
            if m and m[-1][1] == s:
                m[-1] = (m[-1][0], e)
            else:
                m.append((s, e))
        self.free = m


class _Stop(Exception):
    pass


def build_program(stop=None, dumps=None):
    nc = bass.Bass("TRN2", target_bir_lowering=False)
    dumps = {} if dumps is None else dumps

    def phase_end(name, hf, env):
        if stop is not None and stop == (name, hf):
            for dn, (fn_ap, shape) in env.items():
                dt_ = nc.dram_tensor("dbg_" + dn, list(shape), F32, kind="ExternalOutput").ap()
                t_, bufs_ = fn_ap
                final_ops.append(P.dma('pool', lambda e, dt_=dt_, t_=t_: e.dma_start(out=dt_, in_=t_), "dbg", reads=bufs_))
                dumps[dn] = shape
            raise _Stop()

    def din(name, shape):
        return nc.dram_tensor(name, list(shape), F32, kind="ExternalInput").ap()

    xh = din("xh", [2304, D])
    ctxd = din("ctx", [256, D])
    ccols = din("ccols", [128, 16])
    w_ada = din("w_ada", [D, 6 * D])
    b_ada = din("b_ada", [1, 6 * D])
    n1g = din("n1g", [1, D])
    n2g = din("n2g", [1, D])
    fg = din("fg", [1, D])
    w_in = din("w_in", [D, 4608])
    b_in = din("b_in", [1, 4608])
    b_in_cols = din("b_in_cols", [128, 24])
    sink = din("sink", [1, 16])
    w_pool = din("w_pool", [4, 256, 256])
    pscale_cols = din("pscale_cols", [128, 8])
    w_abr = din("w_abr", [D, D])
    w_pbr = din("w_pbr", [D, D])
    w_outd = din("w_out", [D, D])
    w_router = din("w_router", [D, NE])
    b_router = din("b_router", [1, NE])
    w_gu = din("w_gu", [NE, D, 2 * D])
    b_gu_cols = din("b_gu_cols", [128, NE * 16])
    w_down = din("w_down", [NE, D, D])
    b_down = din("b_down", [NE, D])
    identd = din("ident", [128, 128])
    masksd = din("masks", [128, 4 * 512])
    ropec = din("ropec", [128, 18 * 64])
    ropes = din("ropes", [128, 18 * 64])
    pcorr = din("pcorr", [1, 2 * 4 * 16])
    uval = din("uval", [1, 4])
    rowmaskd = din("rowmask", [128, 2])
    m4d = din("m4", [128, 512])
    outd = nc.dram_tensor("out", [2048, D], F32, kind="ExternalOutput").ap()
    modscr = nc.dram_tensor("modscr", [2, 6 * D], F32, kind="Internal").ap()
    dbg_out = {}

    P = Prog()
    ar = Arena(nc, 196608 - 16512 - 64)
    A = ar.alloc
    banks = [nc.psum_tensor("bank%d" % i, [128, 512], F32).__enter__() for i in range(8)]
    PB = [Buf("bank%d" % i) for i in range(8)]

    def bk(i):
        return banks[i][:]

    def bkb(i):
        return banks[i][:].bitcast(BF16)

    def C(eng, fn, r=(), w=()):
        return P.c(eng, fn, reads=r, writes=w)

    def mmf(out, lhsT, rhs, start=True, stop=True):
        f = lambda e: e.matmul(out, lhsT=lhsT, rhs=rhs, start=start, stop=stop)
        f.stop = bool(stop)
        return f

    def DMA(eng, out, in_, dsem, r=(), w=()):
        return P.dma(eng, lambda e: e.dma_start(out=out, in_=in_), dsem, reads=r, writes=w)

    def load(t, in_, cast=False, flat=False):
        return DMA('pool' if cast else 'sp', t.flat if flat else t.ap, in_, "L_" + t.name, w=t.b)

    final_ops = []
    modB = [[Buf("mod%d_%d" % (w, j)) for j in range(6)] for w in range(2)]
    x1B = [Buf("x1d%d" % i) for i in range(16)]

    try:
        ident = A("ident", [128], BF16)
        load(ident, identd[:, :], cast=True)
        onesb = A("onesb", [128], BF16)
        C('pool', lambda e: e.memset(onesb.ap, 1.0), w=onesb.b)
        onesf = A("onesf", [128], F32)
        C('pool', lambda e: e.memset(onesf.ap, 1.0), w=onesf.b)
        epst = A("eps", [1], F32)
        C('pool', lambda e: e.memset(epst.ap, 1e-5), w=epst.b)
        bcols = A("bcols", [24], F32)
        load(bcols, b_in_cols[:, :])
        pscol = A("pscol", [8], F32)
        load(pscol, pscale_cols[:, :])
        pcor = A("pcor", [2, 4, 16], F32)
        load(pcor, pcorr.partition_broadcast(128), flat=True)
        uv = A("uval", [4], F32)
        load(uv, uval.partition_broadcast(128))
        brt = A("brt", [NE], F32)
        load(brt, b_router.partition_broadcast(128))
        wr = A("wr", [8, NE], BF16)
        load(wr, w_router.rearrange("(k p) n -> p k n", p=128), cast=True)
        bguc = A("bguc", [NE, 16], F32)
        load(bguc, b_gu_cols.rearrange("p (a b) -> p a b", a=NE))
        C('dve', lambda e: e.tensor_scalar(out=bguc.ap[:, :, 8:16], in0=bguc.ap[:, :, 8:16], scalar1=1.0, scalar2=None,
                                           op0=ALU.add), r=bguc.b, w=bguc.b)
        bdn = A("bdn", [D], BF16)
        C('pool', lambda e: e.memset(bdn.ap, 0.0), w=bdn.b)
        DMA('pool', bdn.ap[0:NE, :], b_down[:, :], "L_bdn", w=bdn.b)
        cc = A("cc", [8, 2], F32)
        load(cc, ccols.rearrange("p (k w) -> p k w", w=2))
        scb = A("scb", [8, 2], BF16)
        C('act', lambda e: e.activation(out=scb.ap, in_=cc.ap, func=AF.Silu), r=cc.b, w=scb.b)
        badb = A("badb", [6 * D], F32, parts=2)
        load(badb, b_ada.partition_broadcast(2))
        ng = A("ng", [2, D], F32, parts=2)
        DMA('sp', ng.ap[:, 0, :], n1g.partition_broadcast(2), "L_ng0", w=ng.b)
        DMA('sp', ng.ap[:, 1, :], n2g.partition_broadcast(2), "L_ng1", w=ng.b)
        wad = A("wad", [2, 8, D], BF16, nb=2)
        mrow = A("mrow", [2, D], F32, nb=2, parts=2)
        for j in range(6):
            s = j % 2
            DMA('pool', wad.ap[:, s], w_ada[:, j * D:(j + 1) * D].rearrange("(k p) n -> p k n", p=128),
                "L_%s_%d" % (wad.name, s), w=[wad.b[s]])
            for hh in range(2):
                for k in range(8):
                    C('pe', mmf(banks[hh][0:2, :], scb.ap[:, k, :], wad.ap[:, s, k, hh * 512:(hh + 1) * 512],
                                k == 0, k == 7), r=[wad.b[s]] + scb.b, w=[PB[hh]])
                C('dve', lambda e, hh=hh, s=s, j=j: e.tensor_tensor(
                    out=mrow.ap[:, s, hh * 512:(hh + 1) * 512], in0=banks[hh][0:2, :],
                    in1=badb.ap[:, j * D + hh * 512: j * D + (hh + 1) * 512], op=ALU.add),
                  r=[PB[hh]] + badb.b, w=[mrow.b[s]])
            if j in (1, 4):
                gi = 0 if j == 1 else 1
                C('dve', lambda e, s=s, gi=gi: e.scalar_tensor_tensor(
                    out=mrow.ap[:, s, :], in0=mrow.ap[:, s, :], scalar=1.0, in1=ng.ap[:, gi, :],
                    op0=ALU.add, op1=ALU.mult), r=[mrow.b[s]] + ng.b, w=[mrow.b[s]])
            DMA('sp', modscr[:, j * D:(j + 1) * D], mrow.ap[:, s, :], "S_%s_%d" % (mrow.name, s), r=[mrow.b[s]],
                w=[modB[0][j], modB[1][j]])
        ar.release(cc, scb, badb, ng, wad, mrow)

        def load_mod(name, wsel, j):
            t = A(name, [D], F32)
            DMA('sp', t.ap, modscr[wsel:wsel + 1, j * D:(j + 1) * D].partition_broadcast(128), "L_" + t.name,
                r=[modB[wsel][j]], w=t.b)
            return t

        def norm_mod_T(src_ap, src_bufs, Ab, shb, stat, si, tmpf, tmpb, trb, dstT_ap, dst_bufs):
            ssq = stat.ap[:, si, 0:1]
            rstd = stat.ap[:, si, 1:2]
            sb = [stat.b[si]]
            C('act', lambda e: e.activation(out=tmpf[0], in_=src_ap, func=AF.Square, accum_out=ssq),
              r=src_bufs, w=[tmpf[1]] + sb)
            C('act', lambda e: e.activation(out=rstd, in_=ssq, func=AF.Sqrt, scale=1.0 / D, bias=epst.ap[:, 0:1]),
              r=sb + epst.b, w=sb)
            C('dve', lambda e: e.reciprocal(out=rstd, in_=rstd), r=sb, w=sb)
            C('dve', lambda e: e.scalar_tensor_tensor(out=tmpf[0], in0=src_ap, scalar=rstd, in1=Ab.ap,
                                                      op0=ALU.mult, op1=ALU.mult),
              r=list(src_bufs) + sb + Ab.b, w=[tmpf[1]])
            if shb is not None:
                C('dve', lambda e: e.tensor_tensor(out=tmpb[0], in0=tmpf[0], in1=shb.ap, op=ALU.add),
                  r=[tmpf[1]] + shb.b, w=[tmpb[1]])
            else:
                return
            for k in range(8):
                C('pe', lambda e, k=k: e.transpose(out=bkb(trb)[:, k * 128:(k + 1) * 128],
                                                   in_=tmpb[0][:, k * 128:(k + 1) * 128], identity=ident.ap),
                  r=[tmpb[1]] + ident.b, w=[PB[trb]])
            C('act', lambda e: e.activation(out=dstT_ap, in_=bkb(trb).rearrange("p (k t) -> p k t", k=8), func=AF.Copy),
              r=[PB[trb]], w=dst_bufs)

        def tiles_of(a, b):
            return list(range(a // 128, (b - 1) // 128 + 1))

        for hf in range(2):
            hbase = hf * HALF
            A1b = load_mod("A1b", 0, 1)
            sh1b = load_mod("sh1b", 0, 0)
            A1cb = load_mod("A1cb", 1, 1)
            sh1cb = load_mod("sh1cb", 1, 0)
            hT = A("hT", [8, NTH * 128], BF16, nb=NTH, top=True)
            hcT = A("hcT", [8, 256], BF16, nb=2)
            xs1 = A("xs1", [2, D], F32, nb=2)
            tf1 = A("tf1", [2, D], F32, nb=2)
            tb1 = A("tb1", [2, D], BF16, nb=2)
            st1 = A("st1", [NTH + 2, 2], F32, nb=NTH + 2)
            for i in range(NTH + 2):
                s = i % 2
                if i < NTH:
                    src = xh[hbase + i * 128: hbase + (i + 1) * 128, :]
                    dst, dbuf, Ab, shb = hT.ap[:, :, i * 128:(i + 1) * 128], [hT.b[i]], A1b, sh1b
                else:
                    ci = i - NTH
                    src = ctxd[ci * 128:(ci + 1) * 128, :]
                    dst, dbuf, Ab, shb = hcT.ap[:, :, ci * 128:(ci + 1) * 128], [hcT.b[ci]], A1cb, sh1cb
                DMA('sp', xs1.ap[:, s], src, "L_%s_%d" % (xs1.name, s), w=[xs1.b[s]])
                norm_mod_T(xs1.ap[:, s], [xs1.b[s]], Ab, shb, st1, i, (tf1.ap[:, s], tf1.b[s]), (tb1.ap[:, s], tb1.b[s]),
                           6 + s, dst, dbuf)
            ar.release(A1b, sh1b, A1cb, sh1cb, xs1, tf1, tb1, st1)
            phase_end('n1', hf, {'hT': ((hT.ap, hT.b), [128, 8, NTH * 128]), 'hcT': ((hcT.ap, hcT.b), [128, 8, 256])})

            poolT = A("poolT", [8, HALF], BF16, nb=8, top=True)
            NU = HALF + 16
            uT = A("uT", [2, NU], F32)
            T1 = A("T1", [2, NU], F32)
            T2 = A("T2", [2, NU], F32)
            dT = A("dT", [2, HALF], BF16)
            wu = A("wu", [2, 8, 256], BF16, nb=2)
            wpool = A("wpool", [4, 2, 256], BF16)
            load(wpool, w_pool.rearrange("g (kc p) n -> p g kc n", p=128), cast=True)
            for g in range(4):
                s = g % 2
                DMA('pool', wu.ap[:, s], w_in[:, 1536 + g * 256: 1536 + (g + 1) * 256].rearrange("(k p) n -> p k n", p=128),
                    "L_%s_%d" % (wu.name, s), w=[wu.b[s]])
                bi = 0
                for mc in range(2):
                    for (a, b) in ((0, 512), (512, 1024), (1024, NU)):
                        pb = bi % 4
                        bi += 1
                        n = b - a
                        for k in range(8):
                            C('pe', mmf(bk(pb)[:, 0:n], wu.ap[:, s, k, mc * 128:(mc + 1) * 128],
                                        hT.ap[:, k, 120 + a:120 + b], k == 0, k == 7),
                              r=[wu.b[s]] + [hT.b[i] for i in tiles_of(120 + a, 120 + b)], w=[PB[pb]])
                        C('act', lambda e, pb=pb, n=n, mc=mc, a=a, b=b, g=g: e.activation(
                            out=uT.ap[:, mc, a:b], in_=bk(pb)[:, 0:n], func=AF.Identity,
                            bias=bcols.ap[:, g * 2 + mc: g * 2 + mc + 1]), r=[PB[pb]] + bcols.b, w=uT.b)
                C('dve', lambda e: e.tensor_scalar(out=uT.ap[:, :, 0:8], in0=uT.ap[:, :, 0:8],
                                                   scalar1=uv.ap[:, 2 * hf:2 * hf + 1], scalar2=None, op0=ALU.mult),
                  r=uT.b + uv.b, w=uT.b)
                C('dve', lambda e: e.tensor_scalar(out=uT.ap[:, :, NU - 8:NU], in0=uT.ap[:, :, NU - 8:NU],
                                                   scalar1=uv.ap[:, 2 * hf + 1:2 * hf + 2], scalar2=None, op0=ALU.mult),
                  r=uT.b + uv.b, w=uT.b)
                cur = uT
                tmps = [T1, T2]
                for n_ in range(1, g + 2):
                    d_ = 2 ** (n_ - 1)
                    lo = 2 ** n_ - 1
                    nxt = tmps[(n_ - 1) % 2]
                    C('dve', lambda e, cur=cur, nxt=nxt, d_=d_, lo=lo: e.tensor_tensor(
                        out=nxt.ap[:, :, lo:NU], in0=cur.ap[:, :, lo:NU], in1=cur.ap[:, :, lo - d_:NU - d_], op=ALU.add),
                      r=cur.b, w=nxt.b)
                    cur = nxt
                hw = 2 ** g
                wwin = 2 * hw
                off = 8 + hw - 1
                C('dve', lambda e, cur=cur, off=off, g=g: e.tensor_tensor(
                    out=cur.ap[:, :, off:off + 8], in0=cur.ap[:, :, off:off + 8],
                    in1=pcor.ap[:, hf, g, 0:8].unsqueeze(1).broadcast_to([128, 2, 8]), op=ALU.mult),
                  r=cur.b + pcor.b, w=cur.b)
                C('dve', lambda e, cur=cur, off=off, g=g: e.tensor_tensor(
                    out=cur.ap[:, :, off + HALF - 8:off + HALF], in0=cur.ap[:, :, off + HALF - 8:off + HALF],
                    in1=pcor.ap[:, hf, g, 8:16].unsqueeze(1).broadcast_to([128, 2, 8]), op=ALU.mult),
                  r=cur.b + pcor.b, w=cur.b)
                C('dve', lambda e, cur=cur, off=off, wwin=wwin: e.scalar_tensor_tensor(
                    out=dT.ap, in0=cur.ap[:, :, off:off + HALF], scalar=1.0 / wwin, in1=uT.ap[:, :, 8:8 + HALF],
                    op0=ALU.mult, op1=ALU.subtract), r=cur.b + uT.b, w=dT.b)
                for mc in range(2):
                    for tc in range(2):
                        pb = 4 + (mc * 2 + tc) % 2
                        for kc in range(2):
                            C('pe', mmf(bk(pb), wpool.ap[:, g, kc, mc * 128:(mc + 1) * 128],
                                        dT.ap[:, kc, tc * 512:(tc + 1) * 512], kc == 0, kc == 1),
                              r=wpool.b + dT.b, w=[PB[pb]])
                        C('act', lambda e, pb=pb, g=g, mc=mc, tc=tc: e.activation(
                            out=poolT.ap[:, 2 * g + mc, tc * 512:(tc + 1) * 512], in_=bk(pb), func=AF.Identity,
                            scale=pscol.ap[:, 2 * g + mc:2 * g + mc + 1]), r=[PB[pb]] + pscol.b, w=[poolT.b[2 * g + mc]])
            ar.release(uT, T1, T2, dT, wu, wpool)
            phase_end('pool', hf, {'poolT': ((poolT.ap, poolT.b), [128, 8, HALF])})

            masks = A("masks", [4, 512], BF16)
            load(masks, masksd.rearrange("p (a b) -> p a b", a=4), cast=True)
            rc = A("ropec", [18, 64], F32)
            load(rc, ropec.rearrange("p (a b) -> p a b", a=18))
            rs_ = A("ropes", [18, 64], F32)
            load(rs_, ropes.rearrange("p (a b) -> p a b", a=18))
            bqkv = A("bqkv", [1536], F32)
            load(bqkv, b_in[0:1, 0:1536].partition_broadcast(128))
            rowm = A("rowm", [2], F32)
            load(rowm, rowmaskd[:, :])
            m4 = A("m4", [512], F32)
            load(m4, m4d[:, :])
            esk = A("esk", [16], F32)
            load(esk, sink.partition_broadcast(128))
            C('act', lambda e, esk=esk: e.activation(out=esk.ap, in_=esk.ap, func=AF.Exp), r=esk.b, w=esk.b)
            eskB = A("eskB", [16, 128], F32)
            for h in range(16):
                C('dve', lambda e, h=h, esk=esk, eskB=eskB: e.tensor_scalar(
                    out=eskB.ap[:, h, :], in0=onesf.ap, scalar1=esk.ap[:, h:h + 1], scalar2=None, op0=ALU.mult),
                  r=esk.b + onesf.b, w=eskB.b)
            kT = A("kT", [4, NTH * 128], BF16, nb=NTH)
            vd = A("vd", [NTH, 4, 2, 64], BF16, nb=NTH)
            kcT = A("kcT", [4, 256], BF16, nb=2)
            vcd = A("vcd", [2, 4, 2, 64], BF16, nb=2)
            wkv = A("wkv", [8, 512], BF16)
            load(wkv, w_in[:, 1024:1536].rearrange("(k p) n -> p k n", p=128), cast=True)
            kvf = A("kvf", [2, 512], F32, nb=2)
            ra = A("ra", [2, 256], F32, nb=2)
            rb = A("rb", [2, 256], F32, nb=2)
            krd = A("krd", [2, 4, 2, 64], BF16, nb=2)
            for i in range(NTH + 2):
                s = i % 2
                isx = i < NTH
                ci = i - NTH
                gt = hf * 8 + i
                for k in range(8):
                    lh = hT.ap[:, k, i * 128:(i + 1) * 128] if isx else hcT.ap[:, k, ci * 128:(ci + 1) * 128]
                    C('pe', mmf(bk(s), lh, wkv.ap[:, k, :], k == 0, k == 7),
                      r=[hT.b[i] if isx else hcT.b[ci]] + wkv.b, w=[PB[s]])
                C('dve', lambda e, s=s: e.tensor_tensor(out=kvf.ap[:, s], in0=bk(s), in1=bqkv.ap[:, 1024:1536], op=ALU.add),
                  r=[PB[s]] + bqkv.b, w=[kvf.b[s]])
                vdst = vd.ap[:, i] if isx else vcd.ap[:, ci]
                vbuf = [vd.b[i]] if isx else [vcd.b[ci]]
                vsrc = kvf.ap[:, s, 256:512].rearrange("p (g d) -> p g d", g=4)
                C('act', lambda e, vdst=vdst, vsrc=vsrc: e.activation(out=vdst[:, :, 0, :], in_=vsrc, func=AF.Copy),
                  r=[kvf.b[s]], w=vbuf)
                C('act', lambda e, vdst=vdst, vsrc=vsrc: e.activation(out=vdst[:, :, 1, :], in_=vsrc, func=AF.Copy),
                  r=[kvf.b[s]], w=vbuf)
                ksrc = kvf.ap[:, s, 0:256]
                if isx:
                    k3 = ksrc.rearrange("p (g d) -> p g d", g=4)
                    k5 = ksrc.rearrange("p (g a h q) -> p g a h q", g=4, a=2, h=2)
                    ra3 = ra.ap[:, s].rearrange("p (g d) -> p g d", g=4)
                    rb5 = rb.ap[:, s].rearrange("p (g a h q) -> p g a h q", g=4, a=2, h=2)
                    sn4 = rs_.ap[:, gt].rearrange("p (a h q) -> p a h q", a=2, h=2)
                    C('dve', lambda e, k3=k3, ra3=ra3, gt=gt: e.tensor_tensor(
                        out=ra3, in0=k3, in1=rc.ap[:, gt].unsqueeze(1).broadcast_to([128, 4, 64]), op=ALU.mult),
                      r=[kvf.b[s]] + rc.b, w=[ra.b[s]])
                    for hh in range(2):
                        C('dve', lambda e, k5=k5, rb5=rb5, sn4=sn4, hh=hh: e.tensor_tensor(
                            out=rb5[:, :, :, hh, :], in0=k5[:, :, :, 1 - hh, :],
                            in1=sn4[:, :, hh, :].unsqueeze(1).broadcast_to([128, 4, 2, 16]), op=ALU.mult),
                          r=[kvf.b[s]] + rs_.b, w=[rb.b[s]])
                    for dd in range(2):
                        C('dve', lambda e, ra3=ra3, s=s, dd=dd: e.tensor_tensor(
                            out=krd.ap[:, s, :, dd, :], in0=ra3, in1=rb.ap[:, s].rearrange("p (g d) -> p g d", g=4),
                            op=ALU.add), r=[ra.b[s], rb.b[s]], w=[krd.b[s]])
                else:
                    k3 = ksrc.rearrange("p (g d) -> p g d", g=4)
                    for dd in range(2):
                        C('dve', lambda e, k3=k3, s=s, dd=dd: e.tensor_copy(out=krd.ap[:, s, :, dd, :], in_=k3),
                          r=[kvf.b[s]], w=[krd.b[s]])
                trb = 6 + s
                for g in range(4):
                    C('pe', lambda e, g=g, s=s, trb=trb: e.transpose(
                        out=bkb(trb)[:, g * 128:(g + 1) * 128],
                        in_=krd.ap[:, s, g].rearrange("p a d -> p (a d)"), identity=ident.ap),
                      r=[krd.b[s]] + ident.b, w=[PB[trb]])
                kdst = kT.ap[:, :, i * 128:(i + 1) * 128] if isx else kcT.ap[:, :, ci * 128:(ci + 1) * 128]
                C('act', lambda e, kdst=kdst, trb=trb: e.activation(
                    out=kdst, in_=bkb(trb)[:, 0:512].rearrange("p (g t) -> p g t", g=4), func=AF.Copy),
                  r=[PB[trb]], w=[kT.b[i]] if isx else [kcT.b[ci]])
            ar.release(wkv, kvf, ra, rb, krd, hcT)
            phase_end('kv', hf, {'kT': ((kT.ap, kT.b), [128, 4, NTH * 128]), 'vd': ((vd.ap, vd.b), [128, NTH, 4, 2, 64]), 'kcT': ((kcT.ap, kcT.b), [128, 4, 256])})

            attnT = A("attnT", [8, HALF], BF16, nb=NT, top=True)
            wq = A("wq", [8, D], BF16)
            load(wq, w_in[:, 0:1024].rearrange("(k p) n -> p k n", p=128), cast=True)
            qf = A("qf", [D], F32)
            qa = A("qa", [D], F32)
            qb = A("qb", [D], F32)
            qr = A("qr", [D], BF16)
            qT = A("qT", [2, 2, 8, 128], BF16, nb=2)
            prod = A("prod", [512], F32)
            PT = A("PT", [3, 5, 512], BF16, nb=3)
            rden = A("rden", [512], F32)
            sbi = [0]

            def att_prologue(t):
                i = t + 1
                gt = hf * 8 + i
                qs = t % 2
                for hh in range(2):
                    for k in range(8):
                        C('pe', mmf(bk(hh), hT.ap[:, k, i * 128:(i + 1) * 128], wq.ap[:, k, hh * 512:(hh + 1) * 512],
                                    k == 0, k == 7), r=[hT.b[i]] + wq.b, w=[PB[hh]])
                    C('dve', lambda e, hh=hh: e.tensor_tensor(out=qf.ap[:, hh * 512:(hh + 1) * 512], in0=bk(hh),
                                                              in1=bqkv.ap[:, hh * 512:(hh + 1) * 512], op=ALU.add),
                      r=[PB[hh]] + bqkv.b, w=qf.b)
                q3 = qf.ap.rearrange("p (g d) -> p g d", g=16)
                q5 = qf.ap.rearrange("p (g a h q) -> p g a h q", g=16, a=2, h=2)
                qa3 = qa.ap.rearrange("p (g d) -> p g d", g=16)
                qb5 = qb.ap.rearrange("p (g a h q) -> p g a h q", g=16, a=2, h=2)
                sn4 = rs_.ap[:, gt].rearrange("p (a h q) -> p a h q", a=2, h=2)
                C('dve', lambda e, q3=q3, qa3=qa3, gt=gt: e.tensor_tensor(
                    out=qa3, in0=q3, in1=rc.ap[:, gt].unsqueeze(1).broadcast_to([128, 16, 64]), op=ALU.mult),
                  r=qf.b + rc.b, w=qa.b)
                for hh in range(2):
                    C('dve', lambda e, q5=q5, qb5=qb5, sn4=sn4, hh=hh: e.tensor_tensor(
                        out=qb5[:, :, :, hh, :], in0=q5[:, :, :, 1 - hh, :],
                        in1=sn4[:, :, hh, :].unsqueeze(1).broadcast_to([128, 16, 2, 16]), op=ALU.mult),
                      r=qf.b + rs_.b, w=qb.b)
                C('dve', lambda e: e.tensor_tensor(out=qr.ap, in0=qa.ap, in1=qb.ap, op=ALU.add), r=qa.b + qb.b, w=qr.b)
                for c_ in range(8):
                    C('pe', lambda e, c_=c_: e.transpose(out=bkb(7)[:, c_ * 128:(c_ + 1) * 128],
                                                         in_=qr.ap[:, c_ * 128:(c_ + 1) * 128], identity=ident.ap),
                      r=qr.b + ident.b, w=[PB[7]])
                for par in range(2):
                    C('act', lambda e, qs=qs, par=par: e.activation(
                        out=qT.ap[:, qs, par], in_=bkb(7).rearrange("p (k t) -> p k t", k=8), func=AF.Identity,
                        scale=rowm.ap[:, par:par + 1]), r=[PB[7]] + rowm.b, w=[qT.b[qs]])

            def att_A(t, g, ps):
                qs = t % 2
                for kb in range(5):
                    sb_ = 2 + sbi[0] % 2
                    sbi[0] += 1
                    for h in range(4):
                        c_ = 2 * g + h // 2
                        if kb < 3:
                            kl = kT.ap[:, g, (t + kb) * 128:(t + kb + 1) * 128]
                            kbuf = [kT.b[t + kb]]
                        else:
                            kl = kcT.ap[:, g, (kb - 3) * 128:(kb - 2) * 128]
                            kbuf = [kcT.b[kb - 3]]
                        C('pe', mmf(bk(sb_)[:, h * 128:(h + 1) * 128], kl, qT.ap[:, qs, h % 2, c_, :]),
                          r=kbuf + [qT.b[qs]], w=[PB[sb_]])
                    C('act', lambda e, sb_=sb_, ps=ps, kb=kb: e.activation(
                        out=PT.ap[:, ps, kb, :], in_=bk(sb_), func=AF.Exp, scale=0.125), r=[PB[sb_]], w=[PT.b[ps]])
                    if kb in (0, 2):
                        if kb == 0:
                            mi = 2 if (hf == 0 and t == 0) else 0
                        else:
                            mi = 3 if (hf == 1 and t == NT - 1) else 1
                        C('dve', lambda e, ps=ps, kb=kb, mi=mi: e.tensor_tensor(
                            out=PT.ap[:, ps, kb, :], in0=PT.ap[:, ps, kb, :], in1=masks.ap[:, mi, :], op=ALU.mult),
                          r=[PT.b[ps]] + masks.b, w=[PT.b[ps]])

            def att_B(t, g, ps, ub):
                for kb in range(5):
                    if kb < 3:
                        vl = vd.ap[:, t + kb, g].rearrange("p a d -> p (a d)")
                        vbuf = [vd.b[t + kb]]
                    else:
                        vl = vcd.ap[:, kb - 3, g].rearrange("p a d -> p (a d)")
                        vbuf = [vcd.b[kb - 3]]
                    C('pe', mmf(bk(ub), vl, PT.ap[:, ps, kb, :], kb == 0, kb == 4), r=vbuf + [PT.b[ps]], w=[PB[ub]])
                for kb in range(5):
                    C('pe', mmf(bk(6), onesb.ap, PT.ap[:, ps, kb, :], kb == 0, kb == 4),
                      r=onesb.b + [PT.b[ps]], w=[PB[6]])
                C('dve', lambda e, g=g: e.tensor_tensor(
                    out=rden.ap, in0=bk(6), in1=eskB.ap[:, 4 * g:4 * g + 4, :].rearrange("p h q -> p (h q)"),
                    op=ALU.add), r=[PB[6]] + eskB.b, w=rden.b)
                C('dve', lambda e: e.reciprocal(out=rden.ap, in_=rden.ap), r=rden.b, w=rden.b)
                C('dve', lambda e: e.tensor_tensor(out=rden.ap, in0=rden.ap, in1=m4.ap, op=ALU.mult),
                  r=rden.b + m4.b, w=rden.b)
                C('dve', lambda e, ub=ub: e.tensor_tensor(out=prod.ap, in0=bk(ub), in1=rden.ap, op=ALU.mult),
                  r=[PB[ub]] + rden.b, w=prod.b)
                p4 = prod.ap.rearrange("p (c par q) -> p c par q", c=2, par=2)
                C('dve', lambda e, p4=p4, g=g, t=t: e.tensor_tensor(
                    out=attnT.ap[:, 2 * g:2 * g + 2, t * 128:(t + 1) * 128], in0=p4[:, :, 0, :], in1=p4[:, :, 1, :],
                    op=ALU.add), r=prod.b, w=[attnT.b[t]])

            groups = [(t, g) for t in range(NT) for g in range(4)]
            for idx in range(len(groups) + 1):
                if idx < len(groups):
                    t, g = groups[idx]
                    if g == 0:
                        att_prologue(t)
                    att_A(t, g, idx % 3)
                if idx >= 1:
                    t_, g_ = groups[idx - 1]
                    att_B(t_, g_, (idx - 1) % 3, 4 + (idx - 1) % 2)
            phase_end('att', hf, {'attnT': ((attnT.ap, attnT.b), [128, 8, HALF]), 'poolT': ((poolT.ap, poolT.b), [128, 8, HALF]), 'kT': ((kT.ap, kT.b), [128, 4, NTH * 128]), 'vd': ((vd.ap, vd.b), [128, NTH, 4, 2, 64]), 'kcT': ((kcT.ap, kcT.b), [128, 4, 256])})
            ar.release(wq, qf, qa, qb, qr, qT, PT, rden, kT, vd, kcT, vcd, masks, rc, rs_, bqkv, esk, eskB, rowm, m4, prod)

            mT = A("mT", [8, HALF], BF16, nb=NT, top=True)
            wga = A("wga", [8, D], BF16)
            wgp = A("wgp", [8, D], BF16)
            wba = A("wba", [8, D], BF16)
            wbp = A("wbp", [8, D], BF16)
            load(wga, w_in[:, 2560:3584].rearrange("(k p) n -> p k n", p=128), cast=True)
            load(wba, w_abr.rearrange("(k p) n -> p k n", p=128), cast=True)
            load(wgp, w_in[:, 3584:4608].rearrange("(k p) n -> p k n", p=128), cast=True)
            load(wbp, w_pbr.rearrange("(k p) n -> p k n", p=128), cast=True)
            sga = A("sga", [2, 512], F32, nb=2)
            sgp = A("sgp", [2, 512], F32, nb=2)
            t1 = A("t1", [2, 512], F32, nb=2)
            t2 = A("t2", [2, 512], F32, nb=2)
            it = 0
            for tc in range(2):
                hcols = slice(128 + tc * 512, 128 + (tc + 1) * 512)
                hbufs = [hT.b[i] for i in tiles_of(128 + tc * 512, 128 + (tc + 1) * 512)]
                tcs = slice(tc * 512, (tc + 1) * 512)
                tbufs = list(range(tc * 4, tc * 4 + 4))
                for m in range(8):
                    s = it % 2
                    it += 1
                    b0 = 4 * s
                    ms = slice(m * 128, (m + 1) * 128)
                    for k in range(8):
                        C('pe', mmf(bk(b0), wga.ap[:, k, ms], hT.ap[:, k, hcols], k == 0, k == 7),
                          r=wga.b + hbufs, w=[PB[b0]])
                    C('act', lambda e, b0=b0, s=s, m=m: e.activation(out=sga.ap[:, s], in_=bk(b0), func=AF.Sigmoid,
                                                                     bias=bcols.ap[:, 8 + m:9 + m]),
                      r=[PB[b0]] + bcols.b, w=[sga.b[s]])
                    for k in range(8):
                        C('pe', mmf(bk(b0 + 1), wgp.ap[:, k, ms], hT.ap[:, k, hcols], k == 0, k == 7),
                          r=wgp.b + hbufs, w=[PB[b0 + 1]])
                    C('act', lambda e, b0=b0, s=s, m=m: e.activation(out=sgp.ap[:, s], in_=bk(b0 + 1), func=AF.Sigmoid,
                                                                     bias=bcols.ap[:, 16 + m:17 + m]),
                      r=[PB[b0 + 1]] + bcols.b, w=[sgp.b[s]])
                    for k in range(8):
                        C('pe', mmf(bk(b0 + 2), wba.ap[:, k, ms], attnT.ap[:, k, tcs], k == 0, k == 7),
                          r=wba.b + [attnT.b[j] for j in tbufs], w=[PB[b0 + 2]])
                    for k in range(8):
                        C('pe', mmf(bk(b0 + 3), wbp.ap[:, k, ms], poolT.ap[:, k, tcs], k == 0, k == 7),
                          r=wbp.b + poolT.b, w=[PB[b0 + 3]])
                    C('dve', lambda e, b0=b0, s=s: e.tensor_tensor(out=t1.ap[:, s], in0=bk(b0 + 2), in1=sga.ap[:, s],
                                                                   op=ALU.mult), r=[PB[b0 + 2], sga.b[s]], w=[t1.b[s]])
                    C('dve', lambda e, b0=b0, s=s: e.tensor_tensor(out=t2.ap[:, s], in0=bk(b0 + 3), in1=sgp.ap[:, s],
                                                                   op=ALU.mult), r=[PB[b0 + 3], sgp.b[s]], w=[t2.b[s]])
                    C('dve', lambda e, s=s, m=m, tcs=tcs: e.tensor_tensor(out=mT.ap[:, m, tcs], in0=t1.ap[:, s],
                                                                         in1=t2.ap[:, s], op=ALU.add),
                      r=[t1.b[s], t2.b[s]], w=[mT.b[j] for j in tbufs])
            phase_end('br', hf, {'mT': ((mT.ap, mT.b), [128, 8, HALF])})
            ar.release(wga, wgp, wba, wbp, sga, sgp, t1, t2, attnT, poolT, hT)

            G1b = load_mod("G1b", 0, 2)
            A2b = load_mod("A2b", 0, 4)
            sh2b = load_mod("sh2b", 0, 3)
            wo = A("wo", [8, D], BF16)
            load(wo, w_outd.rearrange("(k p) n -> p k n", p=128), cast=True)
            h2T = A("h2T", [8, HALF], BF16, nb=NT, top=True)
            Gt = A("G", [NT, NE], F32, nb=NT, top=True)
            Gs = A("Gs", [NT, NE], F32, nb=NT, top=True)
            x1o = A("x1o", [2, D], F32, nb=2)
            GT = A("GT", [NT, 128], BF16, nb=NT, top=True)
            xs = A("xs", [2, D], F32, nb=2)
            tf = A("tf", [2, D], F32, nb=2)
            tb = A("tb", [2, D], BF16, nb=2)
            st2 = A("st2", [NT, 2], F32, nb=NT)
            lgA = A("lgA", [NT, NE], F32, nb=NT)
            t8A = A("t8A", [NT, 8], F32, nb=NT)
            rwk = A("rwk", [3, NT, NE], F32)
            rsm = A("rsm", [2, NT], F32)
            gbf = A("gbf", [NT, 128], BF16)
            C('pool', lambda e, gbf=gbf: e.memset(gbf.ap, 0.0), w=gbf.b)
            for t in range(NT):
                s = t % 2
                row0 = 128 + hbase + t * 128
                orow = hbase + t * 128
                DMA('sp', xs.ap[:, s], xh[row0:row0 + 128, :], "L_%s_%d" % (xs.name, s), w=[xs.b[s]])
                for hh in range(2):
                    pb = 2 * s + hh
                    hs = slice(hh * 512, (hh + 1) * 512)
                    for k in range(8):
                        C('pe', mmf(bk(pb), mT.ap[:, k, t * 128:(t + 1) * 128], wo.ap[:, k, hs], k == 0, k == 7),
                          r=[mT.b[t]] + wo.b, w=[PB[pb]])
                    C('dve', lambda e, pb=pb, s=s, hs=hs: e.tensor_tensor(out=tf.ap[:, s, hs], in0=bk(pb), in1=G1b.ap[:, hs],
                                                                         op=ALU.mult), r=[PB[pb]] + G1b.b, w=[tf.b[s]])
                    C('dve', lambda e, s=s, hs=hs: e.tensor_tensor(out=x1o.ap[:, s, hs], in0=tf.ap[:, s, hs],
                                                                   in1=xs.ap[:, s, hs], op=ALU.add),
                      r=[tf.b[s], xs.b[s]], w=[x1o.b[s]])
                DMA('sp', outd[orow:orow + 128, :], x1o.ap[:, s], "S_x1", r=[x1o.b[s]], w=[x1B[hf * 8 + t]])
                norm_mod_T(x1o.ap[:, s], [x1o.b[s]], A2b, sh2b, st2, t, (tf.ap[:, s], tf.b[s]), (tb.ap[:, s], tb.b[s]),
                           6 + s, h2T.ap[:, :, t * 128:(t + 1) * 128], [h2T.b[t]])
                for k in range(8):
                    C('pe', mmf(bk(4)[:, 0:NE], h2T.ap[:, k, t * 128:(t + 1) * 128], wr.ap[:, k, :], k == 0, k == 7),
                      r=[h2T.b[t]] + wr.b, w=[PB[4]])
                C('dve', lambda e, t=t: e.tensor_tensor(out=lgA.ap[:, t, :], in0=bk(4)[:, 0:NE], in1=brt.ap, op=ALU.add),
                  r=[PB[4]] + brt.b, w=[lgA.b[t]])
                C('dve', lambda e, t=t: e.max(out=t8A.ap[:, t, :], in_=lgA.ap[:, t, :]), r=[lgA.b[t]], w=[t8A.b[t]])
            mkA, exA, exmA = (rwk.ap[:, j] for j in range(3))
            C('dve', lambda e: e.tensor_tensor(out=mkA, in0=lgA.ap, in1=t8A.ap[:, :, 3:4].broadcast_to([128, NT, NE]),
                                               op=ALU.is_ge), r=lgA.b + t8A.b, w=rwk.b)
            C('dve', lambda e: e.tensor_tensor(out=exA, in0=lgA.ap, in1=t8A.ap[:, :, 0:1].broadcast_to([128, NT, NE]),
                                               op=ALU.subtract), r=lgA.b + t8A.b, w=rwk.b)
            C('act', lambda e: e.activation(out=exA, in_=exA, func=AF.Exp), r=rwk.b, w=rwk.b)
            C('dve', lambda e: e.tensor_tensor(out=exmA, in0=exA, in1=mkA, op=ALU.mult), r=rwk.b, w=rwk.b)
            C('dve', lambda e: e.reduce_sum(out=rsm.ap[:, 0, :], in_=exmA, axis=AX.X), r=rwk.b, w=rsm.b)
            C('dve', lambda e: e.reciprocal(out=rsm.ap[:, 1, :], in_=rsm.ap[:, 0, :]), r=rsm.b, w=rsm.b)
            C('dve', lambda e: e.tensor_tensor(out=Gt.ap, in0=exmA, in1=rsm.ap[:, 1, :].unsqueeze(2).broadcast_to([128, NT, NE]),
                                               op=ALU.mult), r=rwk.b + rsm.b, w=Gt.b)
            C('dve', lambda e: e.tensor_scalar(out=Gs.ap, in0=Gt.ap, scalar1=1.0 / 1.702, scalar2=None, op0=ALU.mult),
              r=Gt.b, w=Gs.b)
            C('dve', lambda e: e.tensor_copy(out=gbf.ap[:, :, 0:NE], in_=Gt.ap), r=Gt.b, w=gbf.b)
            for t in range(NT):
                C('pe', lambda e, t=t: e.transpose(out=bkb(5)[:, t * 128:(t + 1) * 128], in_=gbf.ap[:, t, :], identity=ident.ap),
                  r=gbf.b + ident.b, w=[PB[5]])
            C('act', lambda e: e.activation(out=GT.ap, in_=bkb(5).rearrange("p (t q) -> p t q", t=NT), func=AF.Copy),
              r=[PB[5]], w=GT.b)
            phase_end('out', hf, {'h2T': ((h2T.ap, h2T.b), [128, 8, HALF]), 'G': ((Gt.ap, Gt.b), [128, NT, NE])})
            ar.release(G1b, A2b, sh2b, wo, xs, x1o, tf, tb, st2, lgA, t8A, rwk, rsm, gbf, mT)

            acc = A("acc", [NT, D], F32, nb=NT, top=True)
            wgu = A("wgu", [2, 4, 2, 2 * D], BF16, nb=8)
            wdn = A("wdn", [1, 2, 4, D], BF16, nb=2)
            for t in range(NT):
                for hh in range(2):
                    pb = 4 + (t * 2 + hh) % 4
                    hs = slice(hh * 512, (hh + 1) * 512)
                    C('pe', mmf(bk(pb), GT.ap[:, t, :], bdn.ap[:, hs]), r=[GT.b[t]] + bdn.b, w=[PB[pb]])
                    C('act', lambda e, pb=pb, t=t, hs=hs: e.activation(out=acc.ap[:, t, hs], in_=bk(pb), func=AF.Copy),
                      r=[PB[pb]], w=[acc.b[t]])
            actT = A("actT", [2, 8, 512], BF16, nb=2)
            gc = A("gc", [2, 512], F32, nb=2)
            sg = A("sg", [2, 512], F32, nb=2)
            lc = A("lc", [2, 512], F32, nb=2)
            yi = 0
            pi = 0
            for e_ in range(NEXP):
                es = e_ % 2
                gsrc = w_gu[e_].rearrange("(k p) n -> p k n", p=128)
                dsrc = w_down[e_].rearrange("(k p) n -> p k n", p=128)
                for j in range(4):
                    DMA('pool', wgu.ap[:, es, j], gsrc[:, 2 * j:2 * j + 2, :], "L_%s_%d_%d" % (wgu.name, es, j), w=[wgu.b[es * 4 + j]])
                for j in range(2):
                    DMA('pool', wdn.ap[:, 0, j], dsrc[:, 4 * j:4 * j + 4, :], "L_%s_%d_%d" % (wdn.name, es, j), w=[wdn.b[j]])
                for tc in range(2):
                    as_ = (e_ * 2 + tc) % 2
                    tcs = slice(tc * 512, (tc + 1) * 512)
                    hb_ = [h2T.b[j] for j in range(tc * 4, tc * 4 + 4)]
                    for mp in range(8):
                        s = pi % 2
                        pi += 1
                        bg, bl = 2 * s, 2 * s + 1
                        for k in range(8):
                            C('pe', mmf(bk(bg), wgu.ap[:, es, k // 2, k % 2, mp * 128:(mp + 1) * 128], h2T.ap[:, k, tcs],
                                        k == 0, k == 7), r=[wgu.b[es * 4 + k // 2]] + hb_, w=[PB[bg]])
                        for k in range(8):
                            C('pe', mmf(bk(bl), wgu.ap[:, es, k // 2, k % 2, D + mp * 128:D + (mp + 1) * 128],
                                        h2T.ap[:, k, tcs], k == 0, k == 7), r=[wgu.b[es * 4 + k // 2]] + hb_, w=[PB[bl]])
                        C('dve', lambda e, bg=bg, s=s, e_=e_, mp=mp: e.tensor_scalar(
                            out=gc.ap[:, s], in0=bk(bg), scalar1=bguc.ap[:, e_, mp:mp + 1], scalar2=7.0,
                            op0=ALU.add, op1=ALU.min), r=[PB[bg]] + bguc.b, w=[gc.b[s]])
                        C('act', lambda e, s=s: e.activation(out=sg.ap[:, s], in_=gc.ap[:, s], func=AF.Silu, scale=1.702),
                          r=[gc.b[s]], w=[sg.b[s]])
                        C('dve', lambda e, bl=bl, s=s, e_=e_, mp=mp: e.tensor_scalar(
                            out=lc.ap[:, s], in0=bk(bl), scalar1=bguc.ap[:, e_, 8 + mp:9 + mp], scalar2=8.0,
                            op0=ALU.add, op1=ALU.min), r=[PB[bl]] + bguc.b, w=[lc.b[s]])
                        C('dve', lambda e, s=s, as_=as_, mp=mp: e.scalar_tensor_tensor(
                            out=actT.ap[:, as_, mp, :], in0=lc.ap[:, s], scalar=-6.0, in1=sg.ap[:, s],
                            op0=ALU.max, op1=ALU.mult), r=[lc.b[s], sg.b[s]], w=[actT.b[as_]])
                    for tt in range(4):
                        t = tc * 4 + tt
                        for hh in range(2):
                            pb = 4 + yi % 4
                            yi += 1
                            hs = slice(hh * 512, (hh + 1) * 512)
                            for k in range(8):
                                C('pe', mmf(bk(pb), actT.ap[:, as_, k, tt * 128:(tt + 1) * 128],
                                            wdn.ap[:, 0, k // 4, k % 4, hs], k == 0, k == 7),
                                  r=[actT.b[as_], wdn.b[k // 4]], w=[PB[pb]])
                            C('dve', lambda e, pb=pb, t=t, hs=hs, e_=e_: e.scalar_tensor_tensor(
                                out=acc.ap[:, t, hs], in0=bk(pb), scalar=Gs.ap[:, t, e_:e_ + 1], in1=acc.ap[:, t, hs],
                                op0=ALU.mult, op1=ALU.add), r=[PB[pb], Gs.b[t], acc.b[t]], w=[acc.b[t]])
            phase_end('moe', hf, {'acc': ((acc.ap, acc.b), [128, NT, D])})
            ar.release(wgu, wdn, actT, gc, sg, lc, h2T, Gt, Gs, GT)

            G2b = load_mod("G2b", 0, 5)
            fgb = A("fgb", [D], F32)
            load(fgb, fg.partition_broadcast(128))
            xr = A("xr", [2, D], F32, nb=2)
            xo = A("xo", [2, D], F32, nb=2)
            tfo = A("tfo", [2, D], F32, nb=2)
            st3 = A("st3", [NT, 2], F32, nb=NT)
            for t in range(NT):
                s = t % 2
                orow = hbase + t * 128
                DMA('sp', xr.ap[:, s], outd[orow:orow + 128, :], "L_xr", r=[x1B[hf * 8 + t]], w=[xr.b[s]])
                C('dve', lambda e, s=s, t=t, acc=acc, G2b=G2b, tfo=tfo: e.tensor_tensor(
                    out=tfo.ap[:, s], in0=acc.ap[:, t, :], in1=G2b.ap, op=ALU.mult), r=[acc.b[t]] + G2b.b, w=[tfo.b[s]])
                C('dve', lambda e, s=s, xo=xo, tfo=tfo, xr=xr: e.tensor_tensor(
                    out=xo.ap[:, s], in0=tfo.ap[:, s], in1=xr.ap[:, s], op=ALU.add), r=[tfo.b[s], xr.b[s]], w=[xo.b[s]])
                norm_mod_T(xo.ap[:, s], [xo.b[s]], fgb, None, st3, t, (tfo.ap[:, s], tfo.b[s]), None, None, None, None)
                final_ops.append(DMA('sp', outd[orow:orow + 128, :], tfo.ap[:, s], "S_out", r=[tfo.b[s]], w=[x1B[hf * 8 + t]]))
            phase_end('fin', hf, {'tfin': ((tfo.ap, tfo.b), [128, 2, D]), 'accf': ((acc.ap, acc.b), [128, NT, D])})
            ar.release(G2b, fgb, xr, xo, tfo, st3, acc)

    except _Stop:
        pass
    P.emit(nc, final_ops=final_ops)
    return nc


def build_program_safe(stop=None, dumps=None):
    return build_program(stop, dumps)


def _host_consts(j):
    L = 8192
    ident = np.eye(128, dtype=np.float32)
    jj = np.arange(128)[:, None]
    ii = np.arange(128)[None, :]
    mP = (jj >= ii).astype(np.float32)
    mN = (jj <= ii).astype(np.float32)
    mPF = mP if j != 0 else np.zeros_like(mP)
    mNL = mN if j != 3 else np.zeros_like(mN)
    masks = np.concatenate([np.tile(m, (1, 4)) for m in (mP, mN, mPF, mNL)], axis=1).astype(np.float32)
    pos = j * 2048 - 128 + np.arange(2304)
    posc = np.clip(pos, 0, L - 1)
    row = (posc // 64).astype(np.float32)
    col = (posc % 64).astype(np.float32)
    inv_freq = (np.float32(10000.0) ** (-np.arange(16, dtype=np.float32) / np.float32(16))).astype(np.float32)
    ang = np.stack([row[:, None] * inv_freq, col[:, None] * inv_freq], axis=1).astype(np.float32)
    cs, sn = np.cos(ang).astype(np.float32), np.sin(ang).astype(np.float32)
    cos64 = np.stack([cs, cs], axis=2).reshape(2304, 64)
    sin64 = np.stack([-sn, sn], axis=2).reshape(2304, 64)
    ropec = cos64.reshape(18, 128, 64).transpose(1, 0, 2).reshape(128, 18 * 64)
    ropes = sin64.reshape(18, 128, 64).transpose(1, 0, 2).reshape(128, 18 * 64)
    pcorr = np.ones((2, 4, 16), np.float32)
    uval = np.ones((2, 2), np.float32)
    for hf in range(2):
        p0 = j * 2048 + hf * 1024
        for g in range(4):
            h = 2 ** g
            w = 2 * h
            for side, ts in ((0, range(0, 8)), (1, range(1016, 1024))):
                for q, t in enumerate(ts):
                    p = p0 + t
                    cnt = min(p + h, L) - max(p - h, 0)
                    pcorr[hf, g, side * 8 + q] = w / cnt
        if p0 == 0:
            uval[hf, 0] = 0.0
        if p0 + 1024 == L:
            uval[hf, 1] = 0.0
    rowmask = np.zeros((128, 2), np.float32)
    rowmask[:64, 0] = 1.0
    rowmask[64:, 1] = 1.0
    m4 = np.zeros((128, 4, 128), np.float32)
    for h in range(4):
        m4[:, h, :] = rowmask[:, h % 2][:, None]
    return dict(rowmask=rowmask, m4=m4.reshape(128, 512), ident=ident, masks=masks, ropec=np.ascontiguousarray(ropec), ropes=np.ascontiguousarray(ropes),
                pcorr=pcorr.reshape(1, -1), uval=uval.reshape(1, -1))


def make_in_maps(x, c, ctx, c_ctx, w_ada, b_ada, norm1_g, norm2_g, w_in, b_in, attn_sink, w_pool, pool_scale,
                 w_attn_br, w_pool_br, w_out, w_router, b_router, w_gu, b_gu, w_down, b_down, final_g):
    f = lambda a: np.ascontiguousarray(np.asarray(a, dtype=np.float32))
    x, c, ctx, c_ctx = f(x), f(c), f(ctx), f(c_ctx)
    shared = dict(
        w_ada=f(w_ada[0]), b_ada=f(b_ada[0]).reshape(1, -1), n1g=f(norm1_g[0]).reshape(1, -1),
        n2g=f(norm2_g[0]).reshape(1, -1), fg=f(final_g).reshape(1, -1), w_in=f(w_in[0]),
        b_in=f(b_in[0]).reshape(1, -1), b_in_cols=f(np.asarray(b_in[0])[1536:].reshape(24, 128).T),
        sink=f(attn_sink[0]).reshape(1, -1), w_pool=f(w_pool[0]),
        pscale_cols=f(np.asarray(pool_scale[0]).reshape(8, 128).T), w_abr=f(w_attn_br[0]), w_pbr=f(w_pool_br[0]),
        w_out=f(w_out[0]), w_router=f(w_router[0]), b_router=f(b_router[0]).reshape(1, -1), w_gu=f(w_gu[0]),
        b_gu_cols=f(np.asarray(b_gu[0]).reshape(NE, 16, 128).transpose(2, 0, 1).reshape(128, NE * 16)),
        w_down=f(w_down[0]), b_down=f(b_down[0]))
    maps = []
    for r in range(8):
        b, j = r // 4, r % 4
        xhalo = np.zeros((2304, D), np.float32)
        lo, hi = j * 2048 - 128, (j + 1) * 2048 + 128
        slo, shi = max(lo, 0), min(hi, 8192)
        xhalo[slo - lo: shi - lo] = x[b, slo:shi]
        ccols = np.stack([c[b].reshape(8, 128).T, c_ctx.reshape(8, 128).T], axis=2).reshape(128, 16)
        m = dict(shared)
        m.update(xh=xhalo, ctx=f(ctx[b]), ccols=f(ccols))
        m.update(_host_consts(j))
        maps.append(m)
    return maps


_NC_CACHE = {}


def kernel(**inputs):
    if "nc" not in _NC_CACHE:
        _NC_CACHE["nc"] = build_program()
    nc = _NC_CACHE["nc"]
    maps = make_in_maps(**inputs)
    res = run_bass_kernel_spmd(nc, maps, core_ids=list(range(8)))
    out = np.stack([r["out"] for r in res.results], axis=0)
    return out.reshape(2, 4, 2048, D).reshape(2, 8192, D).astype(np.float32)
```
